# Optimizing a Trainium2 kernel written in Bass

```python
import math
import jax, jax.numpy as jnp
from jax import lax
import numpy as np

D_MODEL = 1024
BATCH = 8
SEQ = 2048
DEPTH = 2

HEAD_DIM = 64
ROPE_THETA = 10000.0
RNN_WIDTH = 1024
RNN_BLOCKS = 16
RNN_BLOCK_W = RNN_WIDTH // RNN_BLOCKS
CONV_W = 4
LRU_C = 8.0
DIFF_HEADS = 4
DIFF_QK = DIFF_HEADS * 2 * HEAD_DIM
DIFF_V = DIFF_HEADS * 2 * HEAD_DIM
MOBA_HEADS = 8
MOBA_W = MOBA_HEADS * HEAD_DIM
MOBA_BLOCK = 256
MOBA_TOPK = 3
MOBA_Q_CHUNK = 16
ATTN_Q_BLOCK = 128
N_BRANCH = 3
IN_SPLITS = (RNN_WIDTH, RNN_WIDTH, DIFF_QK, DIFF_QK, DIFF_V, MOBA_W, MOBA_W, MOBA_W, N_BRANCH * D_MODEL)
IN_COLS = sum(IN_SPLITS)
D_FF = 2816
N_EXPERTS = 8
TOP_K = 2
N_DENSE = (DEPTH + 1) // 2
N_MOE = DEPTH // 2
EPS = 1e-6
NEG = -1e30

kernel_name = "hybrid_gated_rglru_diffattn_moba_moe_block"


def rms_norm(x, g):
    xf = x.astype(jnp.float32)
    y = xf * lax.rsqrt(jnp.mean(xf * xf, axis=-1, keepdims=True) + EPS)
    return (y * g.astype(jnp.float32)).astype(x.dtype)


def rope_tables(positions):
    inv = 1.0 / (ROPE_THETA ** (jnp.arange(0, HEAD_DIM, 2, dtype=jnp.float32) / HEAD_DIM))
    ang = positions.astype(jnp.float32)[..., None] * inv
    return jnp.cos(ang), jnp.sin(ang)


def apply_rope(x, cos, sin):
    xf = x.astype(jnp.float32)
    x1, x2 = jnp.split(xf, 2, axis=-1)
    c = cos[:, :, None, :]
    s = sin[:, :, None, :]
    return jnp.concatenate([x1 * c - x2 * s, x2 * c + x1 * s], axis=-1).astype(x.dtype)


def causal_depthwise_conv(x, w, b):
    C = x.shape[-1]
    y = lax.conv_general_dilated(
        x, w[:, None, :].astype(x.dtype), window_strides=(1,), padding=[(CONV_W - 1, 0)],
        dimension_numbers=("NWC", "WIO", "NWC"), feature_group_count=C)
    return y + b


def rg_lru(x, w_a, b_a, w_x, b_x, lam):
    B, S, _ = x.shape
    xb = x.reshape(B, S, RNN_BLOCKS, RNN_BLOCK_W)
    r = jax.nn.sigmoid(jnp.einsum("bsnd,nde->bsne", xb, w_a).reshape(B, S, RNN_WIDTH) + b_a).astype(jnp.float32)
    i = jax.nn.sigmoid(jnp.einsum("bsnd,nde->bsne", xb, w_x).reshape(B, S, RNN_WIDTH) + b_x).astype(jnp.float32)
    log_a = -LRU_C * r * jax.nn.softplus(-lam.astype(jnp.float32))
    a = jnp.exp(log_a)
    mult = jnp.sqrt(-jnp.expm1(2.0 * log_a))
    u = mult * i * x.astype(jnp.float32)

    def combine(left, right):
        a1, u1 = left
        a2, u2 = right
        return a1 * a2, a2 * u1 + u2

    _, h = lax.associative_scan(combine, (a, u), axis=1)
    return h.astype(x.dtype)


def diff_attention(q, k, v, lam, lam_init, subln_g):
    B, H, _, S, Dh = q.shape
    nq = S // ATTN_Q_BLOCK
    scale = 1.0 / math.sqrt(Dh)
    kf = k.astype(jnp.float32)
    vf = v.astype(jnp.float32)
    qb = jnp.moveaxis(q.reshape(B, H, 2, nq, ATTN_Q_BLOCK, Dh), 3, 0)
    k_pos = jnp.arange(S)

    def block(args):
        qi, i = args
        s = jnp.einsum("bhjqd,bhjkd->bhjqk", qi.astype(jnp.float32), kf) * scale
        q_pos = i * ATTN_Q_BLOCK + jnp.arange(ATTN_Q_BLOCK)
        s = jnp.where(k_pos[None, :] <= q_pos[:, None], s, NEG)
        p = jax.nn.softmax(s, axis=-1)
        a = p[:, :, 0] - lam * p[:, :, 1]
        return jnp.einsum("bhqk,bhkd->bhqd", a, vf)

    o = lax.map(block, (qb, jnp.arange(nq)))
    o = jnp.moveaxis(o, 0, 2).reshape(B, H, S, 2 * Dh)
    o = rms_norm(o, subln_g) * (1.0 - lam_init)
    return o.transpose(0, 2, 1, 3).reshape(B, S, H * 2 * Dh).astype(v.dtype)


def moba_attention(q, k, v):
    B, H, S, Dh = q.shape
    scale = 1.0 / math.sqrt(Dh)
    nb = -(-S // MOBA_BLOCK)
    pad = nb * MOBA_BLOCK - S
    kf = jnp.pad(k.astype(jnp.float32), ((0, 0), (0, 0), (0, pad), (0, 0)))
    vf = jnp.pad(v.astype(jnp.float32), ((0, 0), (0, 0), (0, pad), (0, 0)))
    k_blk = kf.reshape(B, H, nb, MOBA_BLOCK, Dh)
    v_blk = vf.reshape(B, H, nb, MOBA_BLOCK, Dh)
    k_mean = jnp.mean(k_blk, axis=3)
    n_sel = min(MOBA_TOPK, nb - 1)
    nq = S // MOBA_Q_CHUNK
    qc = jnp.moveaxis(q.reshape(B, H, nq, MOBA_Q_CHUNK, Dh), 2, 0)
    bi = jnp.arange(B)[:, None, None, None]
    hi = jnp.arange(H)[None, :, None, None]
    blk_ids = jnp.arange(nb)

    def chunk(args):
        qi, i = args
        qi = qi.astype(jnp.float32)
        q_pos = i * MOBA_Q_CHUNK + jnp.arange(MOBA_Q_CHUNK)
        cur = (i * MOBA_Q_CHUNK) // MOBA_BLOCK
        k_own = lax.dynamic_index_in_dim(k_blk, cur, axis=2, keepdims=False)
        v_own = lax.dynamic_index_in_dim(v_blk, cur, axis=2, keepdims=False)
        own_pos = cur * MOBA_BLOCK + jnp.arange(MOBA_BLOCK)
        s_own = jnp.einsum("bhqd,bhkd->bhqk", qi, k_own) * scale
        s_own = jnp.where(own_pos[None, :] <= q_pos[:, None], s_own, NEG)
        if n_sel == 0:
            p_own = jax.nn.softmax(s_own, axis=-1)
            return jnp.einsum("bhqk,bhkd->bhqd", p_own, v_own)
        gate = jnp.einsum("bhqd,bhnd->bhqn", qi, k_mean)
        gate = jnp.where(blk_ids < cur, gate, NEG)
        _, idx = lax.top_k(gate, n_sel)
        valid = idx < cur
        kg = k_blk[bi, hi, idx]
        vg = v_blk[bi, hi, idx]
        s_sel = jnp.einsum("bhqd,bhqnkd->bhqnk", qi, kg) * scale
        s_sel = jnp.where(valid[..., None], s_sel, NEG).reshape(B, H, MOBA_Q_CHUNK, n_sel * MOBA_BLOCK)
        p = jax.nn.softmax(jnp.concatenate([s_sel, s_own], axis=-1), axis=-1)
        p_sel = p[..., : n_sel * MOBA_BLOCK].reshape(B, H, MOBA_Q_CHUNK, n_sel, MOBA_BLOCK)
        p_own = p[..., n_sel * MOBA_BLOCK:]
        return (jnp.einsum("bhqnk,bhqnkd->bhqd", p_sel, vg)
                + jnp.einsum("bhqk,bhkd->bhqd", p_own, v_own))

    o = lax.map(chunk, (qc, jnp.arange(nq)))
    o = jnp.moveaxis(o, 0, 2).reshape(B, H, S, Dh)
    return o.transpose(0, 2, 1, 3).reshape(B, S, H * Dh).astype(v.dtype)


def token_mixers(h, cos, sin, lam_init, w_in, gate_b, conv_w, conv_b, lru_wa, lru_ba, lru_wx, lru_bx,
                 lru_lambda, diff_qn, diff_kn, diff_lq1, diff_lk1, diff_lq2, diff_lk2, diff_subln,
                 moba_qn, moba_kn, w_br_a, w_br_b, w_br_c, w_out):
    B, S, _ = h.shape
    offsets = [sum(IN_SPLITS[:n]) for n in range(1, len(IN_SPLITS))]
    proj = h @ w_in
    x_rnn, g_rnn, dq, dk, dv, mq, mk, mv, g_br = jnp.split(proj, offsets, axis=-1)

    xc = causal_depthwise_conv(x_rnn, conv_w, conv_b)
    y_a = jax.nn.gelu(g_rnn) * rg_lru(xc, lru_wa, lru_ba, lru_wx, lru_bx, lru_lambda)

    dq = apply_rope(rms_norm(dq.reshape(B, S, 2 * DIFF_HEADS, HEAD_DIM), diff_qn), cos, sin)
    dk = apply_rope(rms_norm(dk.reshape(B, S, 2 * DIFF_HEADS, HEAD_DIM), diff_kn), cos, sin)
    dq = dq.reshape(B, S, DIFF_HEADS, 2, HEAD_DIM).transpose(0, 2, 3, 1, 4)
    dk = dk.reshape(B, S, DIFF_HEADS, 2, HEAD_DIM).transpose(0, 2, 3, 1, 4)
    dv = dv.reshape(B, S, DIFF_HEADS, 2 * HEAD_DIM).transpose(0, 2, 1, 3)
    lam = (jnp.exp(jnp.sum(diff_lq1.astype(jnp.float32) * diff_lk1.astype(jnp.float32)))
           - jnp.exp(jnp.sum(diff_lq2.astype(jnp.float32) * diff_lk2.astype(jnp.float32))) + lam_init)
    y_b = diff_attention(dq, dk, dv, lam, lam_init, diff_subln)

    mq = apply_rope(rms_norm(mq.reshape(B, S, MOBA_HEADS, HEAD_DIM), moba_qn), cos, sin).transpose(0, 2, 1, 3)
    mk = apply_rope(rms_norm(mk.reshape(B, S, MOBA_HEADS, HEAD_DIM), moba_kn), cos, sin).transpose(0, 2, 1, 3)
    mv = mv.reshape(B, S, MOBA_HEADS, HEAD_DIM).transpose(0, 2, 1, 3)
    y_c = moba_attention(mq, mk, mv)

    gates = jax.nn.sigmoid(g_br.reshape(B, S, N_BRANCH, D_MODEL) + gate_b)
    merged = (gates[:, :, 0] * (y_a @ w_br_a) + gates[:, :, 1] * (y_b @ w_br_b)
              + gates[:, :, 2] * (y_c @ w_br_c))
    return merged @ w_out


def swiglu(h, w1, w3, w2):
    return (jax.nn.silu(h @ w1) * (h @ w3)) @ w2


def moe_ffn(h, router_w, router_b, w1, w3, w2):
    logits = (h @ router_w).astype(jnp.float32) + router_b.astype(jnp.float32)
    top_v, top_i = lax.top_k(logits, TOP_K)
    top_w = jax.nn.softmax(top_v, axis=-1)
    combine = jnp.sum(jax.nn.one_hot(top_i, N_EXPERTS, dtype=jnp.float32) * top_w[..., None], axis=-2)
    combine = combine.astype(h.dtype)
    y = jnp.zeros_like(h)
    for e in range(N_EXPERTS):
        y = y + combine[..., e:e + 1] * swiglu(h, w1[e], w3[e], w2[e])
    return y


def setup_inputs(seed: int = 0) -> dict:
    key = jax.random.key(seed)
    counter = [0]

    def nrm(shape, scale):
        counter[0] += 1
        return scale * jax.random.normal(jax.random.fold_in(key, counter[0]), shape, jnp.float32)

    L, D = DEPTH, D_MODEL
    x = nrm((BATCH, SEQ, D), 1.0)
    c = nrm((BATCH, D), 1.0)
    offs = jax.random.randint(jax.random.fold_in(key, 1000), (BATCH, 1), 0, 4096, dtype=jnp.int32)
    positions = offs + jnp.arange(SEQ, dtype=jnp.int32)[None, :]
    u = jax.random.uniform(jax.random.fold_in(key, 1001), (L, RNN_WIDTH), jnp.float32, 0.9, 0.999)
    a0 = u ** (1.0 / LRU_C)
    lru_lambda = jnp.log(a0) - jnp.log1p(-a0)
    return {
        "x": x,
        "c": c,
        "positions": positions,
        "ada_w": nrm((L, D, 6 * D), 0.2 * D ** -0.5),
        "ada_b": nrm((L, 6 * D), 0.02),
        "norm1_g": 1.0 + nrm((L, D), 0.02),
        "norm2_g": 1.0 + nrm((L, D), 0.02),
        "w_in": nrm((L, D, IN_COLS), D ** -0.5),
        "gate_b": nrm((L, N_BRANCH, D), 0.1),
        "conv_w": nrm((L, CONV_W, RNN_WIDTH), CONV_W ** -0.5),
        "conv_b": nrm((L, RNN_WIDTH), 0.02),
        "lru_wa": nrm((L, RNN_BLOCKS, RNN_BLOCK_W, RNN_BLOCK_W), RNN_BLOCK_W ** -0.5),
        "lru_ba": nrm((L, RNN_WIDTH), 0.1),
        "lru_wx": nrm((L, RNN_BLOCKS, RNN_BLOCK_W, RNN_BLOCK_W), RNN_BLOCK_W ** -0.5),
        "lru_bx": nrm((L, RNN_WIDTH), 0.1),
        "lru_lambda": lru_lambda,
        "diff_qn": 1.0 + nrm((L, HEAD_DIM), 0.02),
        "diff_kn": 1.0 + nrm((L, HEAD_DIM), 0.02),
        "diff_lq1": nrm((L, HEAD_DIM), 0.1),
        "diff_lk1": nrm((L, HEAD_DIM), 0.1),
        "diff_lq2": nrm((L, HEAD_DIM), 0.1),
        "diff_lk2": nrm((L, HEAD_DIM), 0.1),
        "diff_subln": 1.0 + nrm((L, 2 * HEAD_DIM), 0.02),
        "moba_qn": 1.0 + nrm((L, HEAD_DIM), 0.02),
        "moba_kn": 1.0 + nrm((L, HEAD_DIM), 0.02),
        "w_br_a": nrm((L, RNN_WIDTH, D), RNN_WIDTH ** -0.5),
        "w_br_b": nrm((L, DIFF_V, D), DIFF_V ** -0.5),
        "w_br_c": nrm((L, MOBA_W, D), MOBA_W ** -0.5),
        "w_out": nrm((L, D, D), D ** -0.5),
        "ffn_w1": nrm((N_DENSE, D, D_FF), D ** -0.5),
        "ffn_w3": nrm((N_DENSE, D, D_FF), D ** -0.5),
        "ffn_w2": nrm((N_DENSE, D_FF, D), D_FF ** -0.5),
        "router_w": nrm((N_MOE, D, N_EXPERTS), D ** -0.5),
        "router_b": nrm((N_MOE, N_EXPERTS), 0.01),
        "moe_w1": nrm((N_MOE, N_EXPERTS, D, D_FF), D ** -0.5),
        "moe_w3": nrm((N_MOE, N_EXPERTS, D, D_FF), D ** -0.5),
        "moe_w2": nrm((N_MOE, N_EXPERTS, D_FF, D), D_FF ** -0.5),
    }


def reference(x, c, positions, ada_w, ada_b, norm1_g, norm2_g, w_in, gate_b, conv_w, conv_b,
              lru_wa, lru_ba, lru_wx, lru_bx, lru_lambda, diff_qn, diff_kn, diff_lq1, diff_lk1,
              diff_lq2, diff_lk2, diff_subln, moba_qn, moba_kn, w_br_a, w_br_b, w_br_c, w_out,
              ffn_w1, ffn_w3, ffn_w2, router_w, router_b, moe_w1, moe_w3, moe_w2):
    cos, sin = rope_tables(positions)
    for l in range(DEPTH):
        mod = (c @ ada_w[l] + ada_b[l])[:, None, :]
        shift1, scale1, gate1, shift2, scale2, gate2 = jnp.split(mod, 6, axis=-1)
        lam_init = 0.8 - 0.6 * math.exp(-0.3 * l)
        h = rms_norm(x, norm1_g[l]) * (1.0 + scale1) + shift1
        mix = token_mixers(h, cos, sin, lam_init, w_in[l], gate_b[l], conv_w[l], conv_b[l],
                           lru_wa[l], lru_ba[l], lru_wx[l], lru_bx[l], lru_lambda[l],
                           diff_qn[l], diff_kn[l], diff_lq1[l], diff_lk1[l], diff_lq2[l], diff_lk2[l],
                           diff_subln[l], moba_qn[l], moba_kn[l], w_br_a[l], w_br_b[l], w_br_c[l], w_out[l])
        x = x + gate1 * mix
        h = rms_norm(x, norm2_g[l]) * (1.0 + scale2) + shift2
        if l % 2 == 0:
            y = swiglu(h, ffn_w1[l // 2], ffn_w3[l // 2], ffn_w2[l // 2])
        else:
            y = moe_ffn(h, router_w[l // 2], router_b[l // 2], moe_w1[l // 2], moe_w3[l // 2], moe_w2[l // 2])
        x = x + gate2 * y
    return x
```

```python
import math
import numpy as np
from contextlib import ExitStack
import concourse.bass as bass
import concourse.mybir as mybir
from concourse.bass_utils import run_bass_kernel_spmd

F32 = mybir.dt.float32
BF16 = mybir.dt.bfloat16
I32 = mybir.dt.int32
AF = mybir.ActivationFunctionType
ALU = mybir.AluOpType
AX = mybir.AxisListType

S = 2048
D = 1024
DFF = 2816
NFC = 22
EPS = 1e-6
BIG = 30000.0


class Reg:
    __slots__ = ("name", "w", "r")

    def __init__(self, name=""):
        self.name = name
        self.w = {}
        self.r = {}


class Op:
    __slots__ = ("eng", "fn", "r", "w", "dma", "waits", "tok", "signal", "val", "idx", "barrier")

    def __init__(self, eng, fn, r, w, dma, barrier=False):
        self.eng = eng
        self.fn = fn
        self.r = r
        self.w = w
        self.dma = dma
        self.waits = []
        self.tok = None
        self.signal = False
        self.val = 0
        self.barrier = barrier


class Prog:
    NDMA = 32

    def __init__(self, nc, es):
        self.nc = nc
        self.ops = []
        self.engs = {"pe": nc.tensor, "act": nc.scalar, "dve": nc.vector, "pool": nc.gpsimd, "sp": nc.sync}
        self.sems = {k: es.enter_context(nc.semaphore("s_" + k)) for k in self.engs}
        self.dsems = [es.enter_context(nc.semaphore("d%d" % i)) for i in range(self.NDMA)]

    def op(self, eng, fn, r=(), w=(), dma=False):
        self.ops.append(Op(eng, fn, list(r), list(w), dma))

    def barrier(self):
        for k in self.engs:
            self.ops.append(Op(k, None, [], [], False, barrier=True))

    def finish(self):
        ops = self.ops
        seen = {k: {} for k in self.engs}
        by_eng = {k: [] for k in self.engs}
        dval = [0] * self.NDMA
        half = self.NDMA // 2
        dnext = {"pool": 0, "other": 0}
        for op in ops:
            X = op.eng
            sx = seen[X]
            waits = op.waits

            def need(tok):
                if tok[0] == "e":
                    _, Y, idx = tok
                    if sx.get(Y, -1) >= idx:
                        return
                    sx[Y] = idx
                    by_eng[Y][idx].signal = True
                    waits.append(tok)
                else:
                    _, s, v = tok
                    if sx.get(("d", s), 0) >= v:
                        return
                    sx[("d", s)] = v
                    waits.append(tok)

            if op.barrier:
                for Y in self.engs:
                    if Y != X and by_eng[Y]:
                        j = len(by_eng[Y]) - 1
                        while j >= 0 and (by_eng[Y][j].dma or by_eng[Y][j].fn is None):
                            j -= 1
                        if j >= 0:
                            need(("e", Y, j))
                for s in range(self.NDMA):
                    if dval[s] > 0:
                        need(("d", s, dval[s]))
                continue
            for reg in op.r:
                for tok in reg.w.values():
                    need(tok)
            for reg in op.w:
                for tok in list(reg.w.values()) + list(reg.r.values()):
                    if tok[0] == "e" and tok[1] == X and not op.dma and X == "pe":
                        continue
                    need(tok)
            op.idx = len(by_eng[X])
            by_eng[X].append(op)
            if op.dma:
                if X == "pool":
                    s = dnext["pool"]
                    dnext["pool"] = (s + 1) % half
                else:
                    s = half + dnext["other"]
                    dnext["other"] = (dnext["other"] + 1) % half
                if dval[s] > 0:
                    need(("d", s, dval[s]))
                dval[s] += 16
                tok = ("d", s, dval[s])
                key = ("d", s)
            else:
                tok = ("e", X, op.idx)
                key = X
            op.tok = tok
            for reg in op.r:
                reg.r[key] = tok
            for reg in op.w:
                reg.w[key] = tok
        cnt = {k: 0 for k in self.engs}
        for op in ops:
            if op.barrier:
                continue
            if (not op.dma) and op.signal:
                cnt[op.eng] += 1
                op.val = cnt[op.eng]
        nwait = 0
        for op in ops:
            e = self.engs[op.eng]
            for tok in op.waits:
                nwait += 1
                if tok[0] == "e":
                    e.wait_ge(self.sems[tok[1]], by_eng[tok[1]][tok[2]].val)
                else:
                    e.wait_ge(self.dsems[tok[1]], tok[2])
            if op.barrier:
                continue
            ins = op.fn(e)
            if op.dma:
                ins.then_inc(self.dsems[op.tok[1]], 16)
            elif op.signal:
                ins.then_inc(self.sems[op.eng], 1)
        sp = self.engs["sp"]
        for s, v in enumerate(dval):
            if v > 0:
                sp.wait_ge(self.dsems[s], v)
        self.stats = (len(ops), nwait, dict(cnt))


class Arena:
    def __init__(self, ap, nwords):
        self.ap = ap
        self.n = nwords
        self.top = 0
        self.peak = 0

    def alloc(self, shape, dt):
        nel = 1
        for s in shape:
            nel *= s
        bpe = 4 if dt in (F32, I32) else 2
        words = (nel * bpe + 3) // 4
        words = (words + 1) // 2 * 2
        assert self.top + words <= self.n, ("arena overflow", self.top, words, self.n)
        v = self.ap[:, self.top:self.top + words]
        self.top += words
        self.peak = max(self.peak, self.top)
        if dt != F32:
            v = v.bitcast(dt)
        v = v[:, 0:nel]
        if len(shape) == 2:
            v = v.rearrange("p (a b) -> p a b", a=shape[0], b=shape[1])
        elif len(shape) == 3:
            v = v.rearrange("p (a b c) -> p a b c", a=shape[0], b=shape[1], c=shape[2])
        return v

    def mark(self):
        return self.top

    def release(self, m):
        self.top = m


C_ID = 0
C_RT = 128
C_OB = 256
C_TRI = 384
C_ONES = 512
C_INVF = 640
C_EPS = 641
C_ONE = 642
C_HALFM = 643
C_NEGM = 644
C_OWN = 708
C_NPI = 772
C_NEGA = 776
C_OWNA = 904
NCONST = 1032

V_G1 = 0
V_G2 = 8
V_ADAB = 16
V_GATEB = 64
V_CONVW = 88
V_CONVB = 120
V_BA = 128
V_BX = 136
V_LAM = 144
V_DQN = 152
V_DKN = 153
V_MQN = 154
V_MKN = 155
V_SUBLN = 156
V_LQK = 160
V_RB = 416
NV = 424


def make_consts():
    c = np.zeros((128, NCONST), np.float32)
    c[:, C_ID:C_ID + 128] = np.eye(128, dtype=np.float32)
    rt = np.zeros((128, 128), np.float32)
    for blk in (0, 64):
        for m in range(64):
            if m < 32:
                rt[blk + m + 32, blk + m] = -1.0
            else:
                rt[blk + m - 32, blk + m] = 1.0
    c[:, C_RT:C_RT + 128] = rt
    ob = np.zeros((128, 128), np.float32)
    ob[0:64, 0:64] = 1.0 / 64
    ob[64:128, 64:128] = 1.0 / 64
    c[:, C_OB:C_OB + 128] = ob
    k = np.arange(128)[:, None]
    q = np.arange(128)[None, :]
    c[:, C_TRI:C_TRI + 128] = (q >= k).astype(np.float32)
    c[:, C_ONES:C_ONES + 128] = 1.0
    inv = (1.0 / (np.float32(10000.0) ** (np.arange(0, 64, 2, dtype=np.float32) / np.float32(64)))).astype(np.float32)
    c[:, C_INVF] = inv[np.arange(128) % 32]
    c[:, C_EPS] = EPS
    c[:, C_ONE] = 1.0
    c[:, C_HALFM] = -0.5
    for cur in range(8):
        for n in range(8):
            c[:, C_NEGM + cur * 8 + n] = -1e30 if n >= cur else 0.0
            c[:, C_OWN + cur * 8 + n] = 1.0 if n == cur else 0.0
    for i in range(16):
        for n in range(8):
            c[:, C_NEGA + i * 8 + n] = -1e30 if n >= i // 2 else 0.0
            c[:, C_OWNA + i * 8 + n] = 1.0 if n == i // 2 else 0.0
    return c


def colform(v, nchunk):
    return np.ascontiguousarray(v.reshape(nchunk, 128).T)


def make_vecs(inp):
    out = np.zeros((2, 128, NV), np.float32)
    for l in range(2):
        o = out[l]
        o[:, V_G1:V_G1 + 8] = colform(inp["norm1_g"][l], 8)
        o[:, V_G2:V_G2 + 8] = colform(inp["norm2_g"][l], 8)
        o[:, V_ADAB:V_ADAB + 48] = colform(inp["ada_b"][l], 48)
        for br in range(3):
            o[:, V_GATEB + br * 8:V_GATEB + br * 8 + 8] = colform(inp["gate_b"][l, br], 8)
        for tap in range(4):
            o[:, V_CONVW + tap * 8:V_CONVW + tap * 8 + 8] = colform(inp["conv_w"][l, tap], 8)
        o[:, V_CONVB:V_CONVB + 8] = colform(inp["conv_b"][l], 8)
        o[:, V_BA:V_BA + 8] = colform(inp["lru_ba"][l], 8)
        o[:, V_BX:V_BX + 8] = colform(inp["lru_bx"][l], 8)
        o[:, V_LAM:V_LAM + 8] = colform(inp["lru_lambda"][l], 8)
        o[:, V_DQN] = np.tile(inp["diff_qn"][l], 2)
        o[:, V_DKN] = np.tile(inp["diff_kn"][l], 2)
        o[:, V_MQN] = np.tile(inp["moba_qn"][l], 2)
        o[:, V_MKN] = np.tile(inp["moba_kn"][l], 2)
        o[:, V_SUBLN] = inp["diff_subln"][l]
        for j, nm in enumerate(["diff_lq1", "diff_lk1", "diff_lq2", "diff_lk2"]):
            o[:, V_LQK + j * 64:V_LQK + j * 64 + 64] = np.broadcast_to(inp[nm][l][None, :], (128, 64))
        o[:, V_RB:V_RB + 8] = np.broadcast_to(inp["router_b"][0][None, :], (128, 8))
    return out


def build_program(stop=None, dbg=None, sub=99):
    nc = bass.Bass("TRN2", target_bir_lowering=False)
    es = ExitStack()
    P = Prog(nc, es)

    def din(name, shape, dt=F32):
        return nc.dram_tensor(name, list(shape), dt, kind="ExternalInput").ap()

    x_d = din("x", [S, D])
    cT_d = din("cT", [128, 8])
    pos_d = din("pos", [128, S], I32)
    consts_d = din("consts", [128, NCONST])
    onehot_d = din("onehot", [8, S])
    vecs_d = din("vecs", [2, 128, NV])
    rw_d = din("rw", [128, 64])
    ada_w = din("ada_w", [2, D, 6 * D])
    w_in = din("w_in", [2, D, 8192])
    lru_wa = din("lru_wa", [2, 16, 64, 64])
    lru_wx = din("lru_wx", [2, 16, 64, 64])
    w_br_a = din("w_br_a", [2, D, D])
    w_br_b = din("w_br_b", [2, 512, D])
    w_br_c = din("w_br_c", [2, 512, D])
    w_out = din("w_out", [2, D, D])
    ffn_w1 = din("ffn_w1", [1, D, DFF])
    ffn_w3 = din("ffn_w3", [1, D, DFF])
    ffn_w2 = din("ffn_w2", [1, DFF, D])
    moe_w1 = din("moe_w1", [1, 8, D, DFF])
    moe_w3 = din("moe_w3", [1, 8, D, DFF])
    moe_w2 = din("moe_w2", [1, 8, DFF, D])
    y_d = nc.dram_tensor("y", [S, D], F32, kind="ExternalOutput").ap()
    dbg_outs = {}

    def dbg_out(name, shape, dt=F32):
        t = nc.dram_tensor("dbg_" + name, list(shape), dt, kind="ExternalOutput").ap()
        dbg_outs[name] = t
        return t

    NW = 53000
    arena_t = es.enter_context(nc.sbuf_tensor("arena", [128, NW], F32))
    A = Arena(arena_t, NW)
    psb = [es.enter_context(nc.psum_tensor("ps%d" % i, [128, 512], F32)) for i in range(8)]
    Rps = [Reg("ps%d" % i) for i in range(8)]

    def mm(out, lhsT, rhs, start, stop, r, w, skip=False):
        if skip:
            P.op("pe", lambda e: e.matmul(out, lhsT=lhsT, rhs=rhs, start=start, stop=stop, skip_group_check=True), r=r, w=w)
        else:
            P.op("pe", lambda e: e.matmul(out, lhsT=lhsT, rhs=rhs, start=start, stop=stop), r=r, w=w)

    def tr(out, in_, ident, r, w):
        P.op("pe", lambda e: e.transpose(out, in_, ident), r=r, w=w)

    def act(out, in_, func, r, w, scale=1.0, bias=None, accum=None):
        def f(e):
            kw = {}
            if bias is not None:
                kw["bias"] = bias
            if accum is not None:
                kw["accum_out"] = accum
            return e.activation(out=out, in_=in_, func=func, scale=scale, **kw)
        P.op("act", f, r=r, w=w)

    def tt(eng, out, in0, in1, op, r, w):
        P.op(eng, lambda e: e.tensor_tensor(out=out, in0=in0, in1=in1, op=op), r=r, w=w)

    def ts(eng, out, in0, s1, s2, op0, op1, r, w, accum=None):
        def f(e):
            if op1 is None:
                return e.tensor_scalar(out=out, in0=in0, scalar1=s1, scalar2=None, op0=op0)
            if accum is not None:
                return e.tensor_scalar(out=out, in0=in0, scalar1=s1, scalar2=s2, op0=op0, op1=op1, accum_out=accum)
            return e.tensor_scalar(out=out, in0=in0, scalar1=s1, scalar2=s2, op0=op0, op1=op1)
        P.op(eng, f, r=r, w=w)

    def stt(out, in0, scalar, in1, op0, op1, r, w, accum=None):
        def f(e):
            if accum is not None:
                return e.scalar_tensor_tensor(out=out, in0=in0, scalar=scalar, in1=in1, op0=op0, op1=op1, accum_out=accum)
            return e.scalar_tensor_tensor(out=out, in0=in0, scalar=scalar, in1=in1, op0=op0, op1=op1)
        P.op("dve", f, r=r, w=w)

    def cp(eng, out, in_, r, w):
        P.op(eng, lambda e: e.tensor_copy(out=out, in_=in_), r=r, w=w)

    def memset(eng, ap, val, w):
        P.op(eng, lambda e: e.memset(ap, val), w=w)

    def dma(eng, out, in_, r, w, accum=False):
        if accum:
            P.op(eng, lambda e: e.dma_start(out=out, in_=in_, accum_op=ALU.add), r=r, w=w, dma=True)
        else:
            P.op(eng, lambda e: e.dma_start(out=out, in_=in_), r=r, w=w, dma=True)

    def kmajor(w2d, c0, ncols):
        return w2d[:, c0:c0 + ncols].rearrange("(k p) n -> p k n", p=128)

    cst = A.alloc([NCONST], F32)
    Rc = Reg("consts")
    vecs = A.alloc([2, NV], F32)
    Rv = Reg("vecs")
    cb16 = A.alloc([5, 128], BF16)
    Rcb = Reg("cb16")
    ident_b = cb16[:, 0, :]
    rt_b = cb16[:, 1, :]
    ob_b = cb16[:, 2, :]
    tri_b = cb16[:, 3, :]
    ones_b = cb16[:, 4, :]
    ident_f = cst[:, C_ID:C_ID + 128]
    ones_f = cst[:, C_ONES:C_ONES + 128]
    eps_c = cst[:, C_EPS:C_EPS + 1]
    one_c = cst[:, C_ONE:C_ONE + 1]
    cTb = A.alloc([8], BF16)
    RcT = Reg("cT")
    modc = A.alloc([2, 48], F32)
    Rmod = Reg("mod")
    AB = A.alloc([2, 4, 8], F32)
    RAB = Reg("AB")
    misc = A.alloc([2, 40], F32)
    Rmisc = Reg("misc")
    M_C1, M_C2, M_NLAM, M_SUBG, M_NBA, M_TMP = 0, 8, 16, 17, 18, 26
    hT = A.alloc([8, S], BF16)
    RhT = [Reg("hT%d" % c) for c in range(4)]
    Ry = [Reg("y%d" % i) for i in range(16)]
    base_mark = A.mark()
    yaT = A.alloc([8, S], BF16)
    RyaT = [Reg("yaT%d" % c) for c in range(2)]
    ybT = A.alloc([4, S], BF16)
    RybT = Reg("ybT")
    ycT = A.alloc([4, S], BF16)
    RycT = Reg("ycT")
    cs_mark = A.mark()
    cosT = A.alloc([S], F32)
    sinT = A.alloc([S], F32)
    Rcs = Reg("cossin")
    A.release(base_mark)

    dma("sp", cst, consts_d[:, :], [], [Rc])
    dma("sp", vecs, vecs_d.rearrange("l p v -> p l v"), [], [Rv])
    for j, c0 in enumerate([C_ID, C_RT, C_OB, C_TRI, C_ONES]):
        dma("pool", cb16[:, j, :], consts_d[:, c0:c0 + 128], [], [Rcb])
    dma("pool", cTb, cT_d[:, :], [], [RcT])

    def compute_rope():
        m0 = A.mark()
        posi = A.alloc([S], I32)
        Rpos = Reg("pos")
        ang = A.alloc([S], F32)
        Rang = Reg("ang")
        kk = A.alloc([S], F32)
        Rkk = Reg("kk")
        rr = A.alloc([S], F32)
        Rrr = Reg("rr")
        dma("sp", posi, pos_d[:, :], [], [Rpos])
        cp("dve", ang, posi, [Rpos], [Rang])
        ts("dve", ang, ang, cst[:, C_INVF:C_INVF + 1], None, ALU.mult, None, [Rang, Rc], [Rang])
        MAGIC = 12582912.0
        C1 = 6.28125
        C2 = 2.0 * math.pi - 6.28125
        for which, dst in ((0, sinT), (1, cosT)):
            src = ang
            if which == 1:
                ts("dve", rr, ang, math.pi / 2, None, ALU.add, None, [Rang], [Rrr])
                src = rr
            ts("dve", kk, src, 1.0 / (2.0 * math.pi), MAGIC, ALU.mult, ALU.add, [Rang, Rrr], [Rkk])
            ts("dve", kk, kk, MAGIC, None, ALU.subtract, None, [Rkk], [Rkk])
            stt(rr, kk, -C1, src, ALU.mult, ALU.add, [Rkk, Rang, Rrr], [Rrr])
            stt(rr, kk, -C2, rr, ALU.mult, ALU.add, [Rkk, Rrr], [Rrr])
            ts("dve", rr, rr, 3.1415925, -3.1415925, ALU.min, ALU.max, [Rrr], [Rrr])
            act(dst, rr, AF.Sin, [Rrr], [Rcs])
        A.release(m0)

    m0 = A.mark()
    adaw = [A.alloc([8, 512], BF16) for _ in range(2)]
    Radaw = [Reg("adaw%d" % i) for i in range(2)]
    it = 0
    for l in range(2):
        for g in range(12):
            buf, Rb = adaw[it % 2], Radaw[it % 2]
            it += 1
            dma("pool", buf, kmajor(ada_w[l], g * 512, 512), [], [Rb])
            for cc in range(4):
                col = g * 4 + cc
                for k in range(8):
                    mm(psb[0][:, l * 48 + col:l * 48 + col + 1], buf[:, k, cc * 128:(cc + 1) * 128], cTb[:, k:k + 1],
                       k == 0, k == 7, [Rb, RcT], [Rps[0]], skip=True)
    for l in range(2):
        tt("dve", modc[:, l, :], psb[0][:, l * 48:(l + 1) * 48], vecs[:, l, V_ADAB:V_ADAB + 48], ALU.add,
           [Rps[0], Rv], [Rmod])
        for wh, (gcol, scq, shq) in enumerate(((V_G1, 1, 0), (V_G2, 4, 3))):
            stt(AB[:, l, 2 * wh, :], modc[:, l, scq * 8:scq * 8 + 8], 1.0, vecs[:, l, gcol:gcol + 8], ALU.add, ALU.mult,
                [Rmod, Rv], [RAB])
            cp("dve", AB[:, l, 2 * wh + 1, :], modc[:, l, shq * 8:shq * 8 + 8], [Rmod], [RAB])
    A.release(m0)

    for l in range(2):
        mt = misc[:, l, M_TMP:M_TMP + 8]
        act(mt, vecs[:, l, V_LAM:V_LAM + 8], AF.Exp, [Rv], [Rmisc], scale=-1.0)
        act(mt, mt, AF.Ln, [Rmisc, Rc], [Rmisc], bias=one_c)
        ts("dve", misc[:, l, M_C1:M_C1 + 8], mt, -8.0, None, ALU.mult, None, [Rmisc], [Rmisc])
        ts("dve", misc[:, l, M_C2:M_C2 + 8], mt, -16.0, None, ALU.mult, None, [Rmisc], [Rmisc])
        lam_init = 0.8 - 0.6 * math.exp(-0.3 * l)
        pr = misc[:, l, M_TMP + 8:M_TMP + 10]
        junk = A.alloc([64], F32)
        for j in range(2):
            stt(junk, vecs[:, l, V_LQK + (2 * j) * 64:V_LQK + (2 * j) * 64 + 64], 1.0,
                vecs[:, l, V_LQK + (2 * j + 1) * 64:V_LQK + (2 * j + 1) * 64 + 64], ALU.mult, ALU.mult,
                [Rv], [Rmisc], accum=pr[:, j:j + 1])
        act(pr, pr, AF.Exp, [Rmisc], [Rmisc])
        tt("dve", misc[:, l, M_NLAM:M_NLAM + 1], pr[:, 1:2], pr[:, 0:1], ALU.subtract, [Rmisc], [Rmisc])
        ts("dve", misc[:, l, M_NLAM:M_NLAM + 1], misc[:, l, M_NLAM:M_NLAM + 1], -lam_init, None, ALU.add, None,
           [Rmisc], [Rmisc])
        ts("dve", misc[:, l, M_SUBG:M_SUBG + 1], vecs[:, l, V_SUBLN:V_SUBLN + 1], 1.0 - lam_init, None, ALU.mult, None,
           [Rv], [Rmisc])
    A.release(base_mark)

    state = {"done": False}

    def stage_end(name):
        if stop == name:
            state["done"] = True
        return state["done"]

    def phase_norm(l, wh, src):
        P.barrier()
        m0 = A.mark()
        NB_ = 4
        xt = [A.alloc([D], F32) for _ in range(NB_)]
        Rxt = [Reg("xt%d" % i) for i in range(NB_)]
        xn = [A.alloc([D], BF16) for _ in range(NB_)]
        Rxn = [Reg("xn%d" % i) for i in range(NB_)]
        junk = A.alloc([D], BF16)
        Rjunk = Reg("junk")
        st = A.alloc([16, 4], F32)
        Rsts = [Reg("st%d" % i) for i in range(16)]
        for i in range(16):
            b = i % NB_
            c = i // 4
            Rst = Rsts[i]
            dma("sp", xt[b], src[i * 128:(i + 1) * 128, :], [Ry[i]], [Rxt[b]])
            stt(junk, xt[b], 1.0, xt[b], ALU.mult, ALU.mult, [Rxt[b]], [Rjunk, Rst], accum=st[:, i, 0:1])
            act(st[:, i, 1:2], st[:, i, 0:1], AF.Ln, [Rst, Rc], [Rst], scale=1.0 / D, bias=eps_c)
            act(st[:, i, 2:3], st[:, i, 1:2], AF.Exp, [Rst], [Rst], scale=-0.5)
            act(xn[b], xt[b], AF.Identity, [Rxt[b], Rst], [Rxn[b]], scale=st[:, i, 2:3])
            pset = (c % 2) * 4
            for k in range(8):
                bank = pset + k // 2
                pv = psb[bank][:, :].bitcast(BF16)
                off = (k % 2) * 512 + (i % 4) * 128
                tr(pv[:, off:off + 128], xn[b][:, k * 128:(k + 1) * 128], ident_b, [Rxn[b], Rcb], [Rps[bank]])
            if i % 4 == 3:
                for k in range(8):
                    bank = pset + k // 2
                    pv = psb[bank][:, :].bitcast(BF16)
                    off = (k % 2) * 512
                    o = hT[:, k, c * 512:(c + 1) * 512]
                    a_col = AB[:, l, 2 * wh, k:k + 1]
                    b_col = AB[:, l, 2 * wh + 1, k:k + 1]
                    if k % 2 == 0:
                        act(o, pv[:, off:off + 512], AF.Identity, [Rps[bank], RAB], [RhT[c]], scale=a_col, bias=b_col)
                    else:
                        ts("dve", o, pv[:, off:off + 512], a_col, b_col, ALU.mult, ALU.add, [Rps[bank], RAB], [RhT[c]])
        A.release(m0)

    def phase_rnn(l):
        P.barrier()
        m0 = A.mark()
        H = 1024
        LW = A.alloc([16, 128], BF16)
        RLW = Reg("LW")
        memset("pool", LW, 0.0, [RLW])
        for j in range(8):
            for g, src in enumerate((lru_wa, lru_wx)):
                for hb in range(2):
                    dma("pool", LW[hb * 64:(hb + 1) * 64, g * 8 + j, hb * 64:(hb + 1) * 64], src[l, 2 * j + hb, :, :], [], [RLW])
        wxg = [A.alloc([2, 8, 128], BF16) for _ in range(8)]
        Rwxg = [Reg("wxg%d" % i) for i in range(8)]
        for j in range(8):
            dma("pool", wxg[j][:, 0, :, :], kmajor(w_in[l], j * 128, 128), [], [Rwxg[j]])
            dma("pool", wxg[j][:, 1, :, :], kmajor(w_in[l], 1024 + j * 128, 128), [], [Rwxg[j]])
        xr = [A.alloc([H + 4], F32) for _ in range(2)]
        Rxr = [Reg("xr%d" % i) for i in range(2)]
        hh = [A.alloc([H], F32) for _ in range(2)]
        Rhh = [Reg("hh%d" % i) for i in range(2)]
        names = ["xc", "r", "ig", "g", "tg", "sg", "a", "a2", "u"]
        B = {n: A.alloc([H], F32) for n in names}
        R = {n: Reg(n) for n in names}
        xcb = A.alloc([H], BF16)
        Rxcb = Reg("xcb")
        vl = lambda c0, j: vecs[:, l, c0 + j:c0 + j + 1]
        it = 0
        for j in range(8):
            wb, Rwb = wxg[j], Rwxg[j]
            for hf in range(2):
                t0 = hf * H
                xb_, Rxb_ = xr[it % 2], Rxr[it % 2]
                xp_, Rxp_ = xr[(it + 1) % 2], Rxr[(it + 1) % 2]
                hb_, Rhb_ = hh[it % 2], Rhh[it % 2]
                hp_, Rhp_ = hh[(it + 1) % 2], Rhh[(it + 1) % 2]
                it += 1
                for g in range(2):
                    for cc in range(2):
                        bank = g * 2 + cc
                        for k in range(8):
                            mm(psb[bank][:, :], wb[:, g, k, :], hT[:, k, t0 + cc * 512:t0 + (cc + 1) * 512], k == 0, k == 7,
                               [Rwb, RhT[(t0 // 512) + cc]], [Rps[bank]])
                if hf == 0:
                    memset("pool", xb_[:, 0:3], 0.0, [Rxb_])
                else:
                    cp("pool", xb_[:, 0:3], xp_[:, H:H + 3], [Rxp_], [Rxb_])
                for cc in range(2):
                    act(xb_[:, 3 + cc * 512:3 + (cc + 1) * 512], psb[cc][:, :], AF.Identity, [Rps[cc]], [Rxb_])
                    act(B["g"][:, cc * 512:(cc + 1) * 512], psb[2 + cc][:, :], AF.Identity, [Rps[2 + cc]], [R["g"]])
                act(B["xc"], xb_[:, 3:3 + H], AF.Identity, [Rxb_, Rv], [R["xc"]], scale=vl(V_CONVW + 3 * 8, j), bias=vl(V_CONVB, j))
                for tap in (2, 1, 0):
                    stt(B["xc"], xb_[:, tap:tap + H], vl(V_CONVW + tap * 8, j), B["xc"], ALU.mult, ALU.add,
                        [Rxb_, R["xc"], Rv], [R["xc"]])
                cp("pool", xcb, B["xc"], [R["xc"]], [Rxcb])
                for g in range(2):
                    for cc in range(2):
                        bank = 4 + g * 2 + cc
                        mm(psb[bank][:, :], LW[:, g * 8 + j, :], xcb[:, cc * 512:(cc + 1) * 512], True, True, [RLW, Rxcb], [Rps[bank]])
                for cc in range(2):
                    sl = slice(cc * 512, (cc + 1) * 512)
                    act(B["r"][:, sl], psb[4 + cc][:, :], AF.Sigmoid, [Rps[4 + cc], Rv], [R["r"]], bias=vl(V_BA, j))
                    act(B["ig"][:, sl], psb[6 + cc][:, :], AF.Sigmoid, [Rps[6 + cc], Rv], [R["ig"]], bias=vl(V_BX, j))
                tt("pool", B["tg"], B["g"], B["g"], ALU.mult, [R["g"]], [R["tg"]])
                ts("pool", B["tg"], B["tg"], 0.044715, 1.0, ALU.mult, ALU.add, [R["tg"]], [R["tg"]])
                tt("pool", B["tg"], B["tg"], B["g"], ALU.mult, [R["tg"], R["g"]], [R["tg"]])
                act(B["sg"], B["tg"], AF.Sigmoid, [R["tg"]], [R["sg"]], scale=1.5957691216057308)
                act(B["a"], B["r"], AF.Exp, [R["r"], Rmisc], [R["a"]], scale=misc[:, l, M_C1 + j:M_C1 + j + 1])
                act(B["a2"], B["r"], AF.Exp, [R["r"], Rmisc], [R["a2"]], scale=misc[:, l, M_C2 + j:M_C2 + j + 1])
                act(B["a2"], B["a2"], AF.Sqrt, [R["a2"], Rc], [R["a2"]], scale=-1.0, bias=one_c)
                tt("dve", B["u"], B["a2"], B["ig"], ALU.mult, [R["a2"], R["ig"]], [R["u"]])
                tt("dve", B["u"], B["u"], B["xc"], ALU.mult, [R["u"], R["xc"]], [R["u"]])
                if hf == 0:
                    P.op("dve", lambda e, o=hb_, a=B["a"], u=B["u"]: e.tensor_tensor_scan(out=o, data0=a, data1=u, initial=0.0, op0=ALU.mult, op1=ALU.add),
                         r=[R["a"], R["u"]], w=[Rhb_])
                else:
                    P.op("dve", lambda e, o=hb_, a=B["a"], u=B["u"], ini=hp_[:, H - 1:H]: e.tensor_tensor_scan(out=o, data0=a, data1=u, initial=ini, op0=ALU.mult, op1=ALU.add),
                         r=[R["a"], R["u"], Rhp_], w=[Rhb_])
                tt("pool", B["sg"], B["sg"], B["g"], ALU.mult, [R["sg"], R["g"]], [R["sg"]])
                tt("dve", yaT[:, j, t0:t0 + H], B["sg"], hb_, ALU.mult, [R["sg"], Rhb_], [RyaT[hf]])
        A.release(m0)

    def seq_gen(gens):
        for g in gens:
            for _ in g:
                yield

    def spaced(g, n):
        for _ in g:
            yield
            for _i in range(n):
                yield

    def qk_gen(l, wt, Rwt, gcol, outs, W, pb, sbk):
        for c in range(4):
            tk = slice(c * 512, (c + 1) * 512)
            for k in range(8):
                mm(psb[pb][:, :], wt[:, k, :], hT[:, k, tk], k == 0, k == 7, [Rwt, RhT[c]], [Rps[pb]])
            yield
            act(W["sqb"], psb[pb][:, :], AF.Square, [Rps[pb]], [W["Rsqb"]])
            yield
            mm(psb[sbk][:, :], ob_b, W["sqb"], True, True, [Rcb, W["Rsqb"]], [Rps[sbk]])
            yield
            act(W["rstd"], psb[sbk][:, :], AF.Ln, [Rps[sbk], Rc], [W["Rrstd"]], bias=eps_c)
            act(W["rstd"], W["rstd"], AF.Exp, [W["Rrstd"]], [W["Rrstd"]], scale=-0.5)
            yield
            stt(W["qn"], psb[pb][:, :], vecs[:, l, gcol:gcol + 1], W["rstd"], ALU.mult, ALU.mult,
                [Rps[pb], Rv, W["Rrstd"]], [W["Rqn"]])
            yield
            act(W["qnb"], W["qn"], AF.Identity, [W["Rqn"]], [W["Rqnb"]])
            yield
            mm(psb[sbk][:, :], rt_b, W["qnb"], True, True, [Rcb, W["Rqnb"]], [Rps[sbk]])
            yield
            tt("dve", W["t2"], psb[sbk][:, :], sinT[:, tk], ALU.mult, [Rps[sbk], Rcs], [W["Rt2"]])
            tt("dve", W["qn"], W["qn"], cosT[:, tk], ALU.mult, [W["Rqn"], Rcs], [W["Rqn"]])
            yield
            for (o_ap, ps_, Ro) in outs(c):
                tt("dve", o_ap, W["qn"][ps_, :], W["t2"][ps_, :], ALU.add, [W["Rqn"], W["Rt2"]], [Ro])
            yield

    def drive(gens):
        gens = list(gens)
        while gens:
            for g in list(gens):
                try:
                    next(g)
                except StopIteration:
                    gens.remove(g)

    def qk_work(tag=""):
        H = 512
        W = {}
        W["sqb"] = A.alloc([H], BF16)
        W["rstd"] = A.alloc([H], F32)
        W["qn"] = A.alloc([H], F32)
        W["qnb"] = A.alloc([H], BF16)
        W["t2"] = A.alloc([H], F32)
        for n in ("sqb", "rstd", "qn", "qnb", "t2"):
            W["R" + n] = Reg(n + tag)
        return W

    def run_pipeline(tiles, S_step, AV_step, Dp, bg=None):
        deferred = []
        n = len(tiles)
        for idx in range(n + Dp):
            if bg is not None:
                next(bg, None)
            if idx < n:
                S_step(idx, tiles[idx])
            if idx >= Dp:
                more = AV_step(idx - Dp, tiles[idx - Dp])
                for (dl, fn) in (more or []):
                    deferred.append((idx + dl, fn))
            due = [f for (t_, f) in deferred if t_ <= idx]
            deferred = [(t_, f) for (t_, f) in deferred if t_ > idx]
            for f in due:
                f()
        for (t_, f) in deferred:
            f()

    def phase_diff(l):
        P.barrier()
        m0 = A.mark()
        W = qk_work()
        W2 = qk_work("k")
        wv = A.alloc([8, 512], BF16)
        Rwv = Reg("wv")
        Vd = A.alloc([16, 4, 130], BF16)
        RVd = Reg("Vd")
        wqk4 = [[A.alloc([8, 128], BF16) for _ in range(2)] for _ in range(2)]
        Rwqk4 = [[Reg("wqk%d_%d" % (i, j_)) for j_ in range(2)] for i in range(2)]
        qT2 = [A.alloc([S], BF16) for _ in range(2)]
        kT2 = [A.alloc([S], BF16) for _ in range(2)]
        RqT2 = [Reg("qT%d" % i) for i in range(2)]
        RkT2 = [Reg("kT%d" % i) for i in range(2)]
        Eb = [A.alloc([256], BF16) for _ in range(3)]
        REb = [Reg("E%d" % i) for i in range(3)]
        Osb = A.alloc([2, 2, 130], F32)
        ROsb = Reg("Osb")
        sm = A.alloc([16], F32)
        Rsm = Reg("sm")
        o0 = A.alloc([128], F32)
        Ro0 = Reg("o0")
        junk = A.alloc([128], F32)
        Rjunk = Reg("junkd")
        onb = A.alloc([128], BF16)
        Ronb = Reg("onb")
        dma("pool", wv, kmajor(w_in[l], 3072, 512), [], [Rwv])
        memset("pool", Vd[:, :, :, 128:129], 1.0, [RVd])
        for i in range(16):
            bank = i % 2
            for k in range(8):
                mm(psb[bank][:, :], hT[:, k, i * 128:(i + 1) * 128], wv[:, k, :], k == 0, k == 7, [RhT[i // 4], Rwv], [Rps[bank]])
            act(Vd[:, i, :, 0:128], psb[bank][:, :].rearrange("p (h d) -> p h d", h=4), AF.Identity, [Rps[bank]], [RVd])
        NS, NE = 4, 4
        RS = [Reg("Sd%d" % i) for i in range(NS)]
        onbs = [A.alloc([128], BF16) for _ in range(6)]
        Ronbs = [Reg("onb%d" % i) for i in range(6)]
        Osb2 = [Osb, A.alloc([2, 2, 130], F32)]
        ROsb2 = [ROsb, Reg("Osb1")]
        sm2 = [sm, A.alloc([16], F32)]
        Rsm2 = [Rsm, Reg("sm1")]
        Eb4 = Eb + [A.alloc([256], BF16)]
        REb4 = REb + [Reg("E3")]
        cnt = {"onb": 0, "pv": 0}
        RpT = [Reg("pT0"), Reg("pT1")]

        def prep_gens(hn, banks_q, banks_k):
            pb_ = hn % 2
            wq_, wk_ = wqk4[pb_]
            Rwq_, Rwk_ = Rwqk4[pb_]
            dma("pool", wq_, kmajor(w_in[l], 2048 + hn * 128, 128), [], [Rwq_])
            dma("pool", wk_, kmajor(w_in[l], 2560 + hn * 128, 128), [], [Rwk_])
            qd, kd, Rqd, Rkd = qT2[pb_], kT2[pb_], RqT2[pb_], RkT2[pb_]
            gq = qk_gen(l, wq_, Rwq_, V_DQN, lambda c: [(qd[:, c * 512:(c + 1) * 512], slice(0, 128), Rqd)], W, banks_q[0], banks_q[1])
            gk = qk_gen(l, wk_, Rwk_, V_DKN, lambda c: [(kd[:, c * 512:(c + 1) * 512], slice(0, 128), Rkd)], W2, banks_k[0], banks_k[1])
            return gq, gk

        drive(prep_gens(0, (0, 2), (1, 3)))
        for h in range(4):
            qT, kT, RqT, RkT = qT2[h % 2], kT2[h % 2], RqT2[h % 2], RkT2[h % 2]
            bg = None
            if h < 3:
                gq_, gk_ = prep_gens(h + 1, (0, 1), (0, 1))
                bg = spaced(seq_gen([gq_, gk_]), 1)
            tiles = []
            for qc in range(8):
                for j in range(2):
                    nk = 2 * qc + 2
                    for kt in range(nk):
                        tiles.append((qc, j, kt, kt == nk - 1))

            def S_step(idx, t, qT=qT, kT=kT, RqT=RqT, RkT=RkT):
                qc, j, kt, lastk = t
                q0 = qc * 256
                k0 = kt * 128
                lo = max(q0, k0)
                ncols = q0 + 256 - lo
                sbank = (4, 5, 2, 3)[idx % NS]
                sps = psb[sbank][:, 0:ncols]
                E, RE = Eb4[idx % NE], REb4[idx % NE]
                rows = slice(64 * j, 64 * j + 64)
                mm(sps, kT[rows, k0:k0 + 128], qT[rows, lo:q0 + 256], True, True, [RkT, RqT], [Rps[sbank]])
                act(E[:, 0:ncols], sps, AF.Exp, [Rps[sbank]], [RE], scale=0.125)
                if k0 >= q0:
                    tt("dve", E[:, 0:128], E[:, 0:128], tri_b, ALU.mult, [RE, Rcb], [RE])

            def AV_step(idx, t, h=h):
                qc, j, kt, lastk = t
                q0 = qc * 256
                k0 = kt * 128
                lo = max(q0, k0)
                E, RE = Eb4[idx % NE], REb4[idx % NE]
                obank = 6 + j
                O_, RO_ = Osb2[qc % 2], ROsb2[qc % 2]
                sm_, Rsm_ = sm2[qc % 2], Rsm2[qc % 2]
                for qt in range(2):
                    qs = q0 + qt * 128
                    if qs < k0:
                        continue
                    off = qs - lo
                    ov = psb[obank][:, qt * 130:qt * 130 + 129]
                    mm(ov, E[:, off:off + 128], Vd[:, kt, h, 0:129], (kt == 0 and qt == 0), kt == (qs // 128),
                       [RE, RVd], [Rps[obank]], skip=True)
                if not lastk:
                    return None
                act(O_[:, j, :, 0:129], psb[obank][:, 0:260].rearrange("p (a b) -> p a b", a=2)[:, :, 0:129], AF.Identity,
                    [Rps[obank]], [RO_])
                if j == 0:
                    return None
                P.op("dve", lambda e, o=sm_[:, 0:4], i=O_[:, :, :, 128]: e.reciprocal(out=o.rearrange("p (a b) -> p a b", a=2), in_=i),
                     r=[RO_], w=[Rsm_])
                ts("dve", sm_[:, 4:6], sm_[:, 2:4], misc[:, l, M_NLAM:M_NLAM + 1], None, ALU.mult, None, [Rsm_, Rmisc], [Rsm_])
                used = []
                for qt in range(2):
                    ob_, Rob_ = onbs[cnt["onb"] % 6], Ronbs[cnt["onb"] % 6]
                    cnt["onb"] += 1
                    used.append((ob_, Rob_))
                    ts("dve", o0, O_[:, 0, qt, 0:128], sm_[:, qt:qt + 1], None, ALU.mult, None, [RO_, Rsm_], [Ro0])
                    stt(o0, O_[:, 1, qt, 0:128], sm_[:, 4 + qt:5 + qt], o0, ALU.mult, ALU.add, [RO_, Rsm_, Ro0], [Ro0])
                    stt(junk, o0, 1.0, o0, ALU.mult, ALU.mult, [Ro0], [Rjunk, Rsm_], accum=sm_[:, 8 + qt:9 + qt])
                    act(sm_[:, 10 + qt:11 + qt], sm_[:, 8 + qt:9 + qt], AF.Ln, [Rjunk, Rsm_, Rc], [Rsm_], scale=1.0 / 128, bias=eps_c)
                    act(sm_[:, 12 + qt:13 + qt], sm_[:, 10 + qt:11 + qt], AF.Exp, [Rsm_], [Rsm_], scale=-0.5)
                    ts("dve", ob_, o0, sm_[:, 12 + qt:13 + qt], None, ALU.mult, None, [Ro0, Rsm_], [Rob_])
                pvi = cnt["pv"] % 2
                cnt["pv"] += 1

                def fin(used=used, pvi=pvi, q0=q0, h=h):
                    pv = psb[1][:, :].bitcast(BF16)
                    for qt in range(2):
                        tr(pv[:, pvi * 256 + qt * 128:pvi * 256 + (qt + 1) * 128], used[qt][0], ident_b, [used[qt][1], Rcb], [Rps[1]])
                    act(ybT[:, h, q0:q0 + 256], pv[:, pvi * 256:(pvi + 1) * 256], AF.Identity, [Rps[1], Rmisc], [RybT],
                        scale=misc[:, l, M_SUBG:M_SUBG + 1])
                return [(4, fin)]

            run_pipeline(tiles, S_step, AV_step, 2, bg=bg)
            if bg is not None:
                for _ in bg:
                    pass
        A.release(m0)

    def phase_moba(l):
        P.barrier()
        m0 = A.mark()
        W = qk_work()
        W2 = qk_work("k")
        wv = A.alloc([8, 512], BF16)
        Rwv = Reg("wvm")
        Vm = A.alloc([16, 8, 66], BF16)
        RVm = Reg("Vm")
        wqk = [A.alloc([8, 128], BF16) for _ in range(2)]
        Rwqk = [Reg("wqkm%d" % i) for i in range(2)]
        Qe = A.alloc([S], BF16)
        Qo = A.alloc([S], BF16)
        Ke = A.alloc([S], BF16)
        Ko = A.alloc([S], BF16)
        RQe, RQo, RKe, RKo = Reg("Qe"), Reg("Qo"), Reg("Ke"), Reg("Ko")
        Eb = [A.alloc([512], BF16) for _ in range(4)]
        REb = [Reg("Em%d" % i) for i in range(4)]
        km = A.alloc([16], F32)
        Rkm = Reg("km")
        kmb = A.alloc([16], BF16)
        Rkmb = Reg("kmb")
        gmA = A.alloc([2, 16, 8], F32)
        Rgm = Reg("gm")
        topA = A.alloc([2, 16, 8], F32)
        Rtop = Reg("top")
        biasA = A.alloc([16, 128], BF16)
        Rbias = Reg("biasA")
        memset("pool", biasA, 0.0, [Rbias])
        yct = A.alloc([16, 128], BF16)
        Ryct = Reg("yct")
        rden2 = [A.alloc([4], F32) for _ in range(2)]
        Rrden2 = [Reg("rden%d" % i) for i in range(2)]
        memset("pool", Qo[0:64, :], 0.0, [RQo])
        memset("pool", Ko[0:64, :], 0.0, [RKo])
        memset("pool", Ke[64:128, :], 0.0, [RKe])
        memset("pool", Qe[64:128, :], 0.0, [RQe])
        dma("pool", Ke[64:72, :], onehot_d[:, :], [], [RKe])
        dma("pool", Ko[0:8, :], onehot_d[:, :], [], [RKo])
        dma("pool", wv, kmajor(w_in[l], 4608, 512), [], [Rwv])
        memset("pool", Vm[:, :, :, 64:65], 1.0, [RVm])
        for i in range(16):
            bank = i % 2
            for k in range(8):
                mm(psb[bank][:, :], hT[:, k, i * 128:(i + 1) * 128], wv[:, k, :], k == 0, k == 7, [RhT[i // 4], Rwv], [Rps[bank]])
            act(Vm[:, i, :, 0:64], psb[bank][:, :].rearrange("p (h d) -> p h d", h=8), AF.Identity, [Rps[bank]], [RVm])
        eit = 0
        if sub <= 0:
            A.release(m0)
            return
        for cpi in range(4):
            dma("pool", wqk[0], kmajor(w_in[l], 3584 + cpi * 128, 128), [], [Rwqk[0]])
            dma("pool", wqk[1], kmajor(w_in[l], 4096 + cpi * 128, 128), [], [Rwqk[1]])
            drive([qk_gen(l, wqk[0], Rwqk[0], V_MQN,
                          lambda c: [(Qe[0:64, c * 512:(c + 1) * 512], slice(0, 64), RQe),
                                     (Qo[64:128, c * 512:(c + 1) * 512], slice(64, 128), RQo)], W, 0, 2),
                   qk_gen(l, wqk[1], Rwqk[1], V_MKN,
                          lambda c: [(Ke[0:64, c * 512:(c + 1) * 512], slice(0, 64), RKe),
                                     (Ko[64:128, c * 512:(c + 1) * 512], slice(64, 128), RKo)], W2, 1, 3)])
            if sub <= 1:
                continue
            P.op("dve", lambda e: e.tensor_reduce(out=km[0:64, 0:8], in_=Ke[0:64, :].rearrange("p (n t) -> p n t", n=8), axis=AX.X, op=ALU.add),
                 r=[RKe], w=[Rkm])
            P.op("dve", lambda e: e.tensor_reduce(out=km[64:128, 0:8], in_=Ko[64:128, :].rearrange("p (n t) -> p n t", n=8), axis=AX.X, op=ALU.add),
                 r=[RKo], w=[Rkm])
            ts("dve", km[:, 0:8], km[:, 0:8], 1.0 / 256, None, ALU.mult, None, [Rkm], [Rkm])
            cp("dve", kmb[:, 0:8], km[:, 0:8], [Rkm], [Rkmb])
            tt("dve", km[:, 8:16], km[:, 0:8], kmb[:, 0:8], ALU.subtract, [Rkm, Rkmb], [Rkm])
            cp("dve", kmb[:, 8:16], km[:, 8:16], [Rkm], [Rkmb])
            if sub <= 2:
                continue
            for i in range(16):
                mm(psb[2][:, i * 16:(i + 1) * 16], Qe[0:64, i * 128:(i + 1) * 128], kmb[0:64, :], True, True, [RQe, Rkmb], [Rps[2]], skip=True)
                mm(psb[1][:, i * 16:(i + 1) * 16], Qo[64:128, i * 128:(i + 1) * 128], kmb[64:128, :], True, True, [RQo, Rkmb], [Rps[1]], skip=True)
            negA = cst[:, C_NEGA:C_NEGA + 128].rearrange("p (i n) -> p i n", i=16)
            ownA = cst[:, C_OWNA:C_OWNA + 128].rearrange("p (i n) -> p i n", i=16)
            for hh_ in range(2):
                gbk = 2 if hh_ == 0 else 1
                G = psb[gbk][:, 0:256].rearrange("p (i c) -> p i c", i=16)
                g3 = gmA[:, hh_]
                tt("dve", g3, G[:, :, 8:16], negA, ALU.add, [Rps[gbk], Rc], [Rgm])
                tt("dve", g3, G[:, :, 0:8], g3, ALU.add, [Rps[gbk], Rgm], [Rgm])
                for i in range(16):
                    P.op("dve", lambda e, o=topA[:, hh_, i, :], i_=g3[:, i, :]: e.max(out=o, in_=i_), r=[Rgm], w=[Rtop])
                tt("dve", g3, g3, topA[:, hh_, :, 2:3].to_broadcast([128, 16, 8]), ALU.is_ge, [Rgm, Rtop], [Rgm])
                tt("dve", g3, g3, ownA, ALU.max, [Rgm, Rc], [Rgm])
                bcol = 64 if hh_ == 0 else 0
                ts("dve", biasA[:, :, bcol:bcol + 8], g3, BIG, -BIG, ALU.mult, ALU.add, [Rgm], [Rbias])
            for i in range(16):
                c4 = i // 4
                sl = slice((i % 4) * 128, (i % 4) * 128 + 128)
                mm(psb[3][:, sl], biasA[:, i, :], ident_b, True, True, [Rbias, Rcb], [Rps[3]], skip=True)
                if i % 4 == 3:
                    act(Qe[64:72, c4 * 512:(c4 + 1) * 512], psb[3][64:72, :], AF.Identity, [Rps[3]], [RQe])
                    act(Qo[0:8, c4 * 512:(c4 + 1) * 512], psb[3][0:8, :], AF.Identity, [Rps[3]], [RQo])
            if sub <= 3:
                continue
            tiles = []
            for par in range(2):
                for qc in range(4):
                    nk = 4 * qc + 4
                    for kt in range(nk):
                        tiles.append((par, qc, kt, kt == nk - 1))

            def S_step(idx, t):
                par, qc, kt, lastk = t
                Qa, Ka, RQa, RKa = (Qe, Ke, RQe, RKe) if par == 0 else (Qo, Ko, RQo, RKo)
                rows = slice(0, 72) if par == 0 else slice(0, 128)
                q0 = qc * 512
                k0 = kt * 128
                lo = max(q0, k0)
                ncols = q0 + 512 - lo
                sbank = (4, 5, 0, 1)[idx % 4]
                E, RE = Eb[idx % 4], REb[idx % 4]
                sps = psb[sbank][:, 0:ncols]
                mm(sps, Ka[rows, k0:k0 + 128], Qa[rows, lo:q0 + 512], True, True, [RKa, RQa], [Rps[sbank]])
                act(E[:, 0:ncols], sps, AF.Exp, [Rps[sbank]], [RE], scale=0.125)
                if k0 >= q0:
                    tt("dve", E[:, 0:128], E[:, 0:128], tri_b, ALU.mult, [RE, Rcb], [RE])

            def AV_step(idx, t, cpi=cpi):
                par, qc, kt, lastk = t
                h = 2 * cpi + par
                q0 = qc * 512
                k0 = kt * 128
                lo = max(q0, k0)
                E, RE = Eb[idx % 4], REb[idx % 4]
                obank = 6 + (qc % 2)
                for qt in range(4):
                    qs = q0 + qt * 128
                    if qs < k0:
                        continue
                    off = qs - lo
                    ov = psb[obank][:, qt * 66:qt * 66 + 65]
                    mm(ov, E[:, off:off + 128], Vm[:, kt, h, 0:65], (kt == 0 and qt == 0), kt == (qs // 128), [RE, RVm], [Rps[obank]], skip=True)
                if not lastk:
                    return None
                ov = psb[obank][:, 0:264].rearrange("p (a b) -> p a b", a=4)
                rd_ = rden2[qc % 2]
                Rrd_ = Rrden2[qc % 2]
                P.op("dve", lambda e, o=rd_, i_=ov[:, :, 64]: e.reciprocal(out=o, in_=i_), r=[Rps[obank]], w=[Rrd_])
                for qt in range(4):
                    ts("dve", yct[:, qc * 4 + qt, par * 64:(par + 1) * 64], ov[:, qt, 0:64], rd_[:, qt:qt + 1], None, ALU.mult, None,
                       [Rps[obank], Rrd_], [Ryct])
                if par == 0:
                    return None

                def fin(qc=qc, q0=q0, cpi=cpi):
                    pv = psb[3][:, :].bitcast(BF16)
                    half = (qc % 2) * 512
                    for qt in range(4):
                        tr(pv[:, half + qt * 128:half + (qt + 1) * 128], yct[:, qc * 4 + qt, :], ident_b, [Ryct, Rcb], [Rps[3]])
                    act(ycT[:, cpi, q0:q0 + 512], pv[:, half:half + 512], AF.Identity, [Rps[3]], [RycT])
                return [(3, fin)]

            run_pipeline(tiles, S_step, AV_step, 2)
        A.release(m0)

    def build_gateB(l, q, gB, RgB):
        dg = A.alloc([128], F32)
        Rdg = Reg("dg")
        for k in range(8):
            ts("dve", dg, ident_f, modc[:, l, q * 8 + k:q * 8 + k + 1], None, ALU.mult, None, [Rc, Rmod], [Rdg])
            mm(psb[k % 2][:, 0:128], ones_f, dg, True, True, [Rc, Rdg], [Rps[k % 2]])
            cp("dve", gB[:, k * 128:(k + 1) * 128], psb[k % 2][:, 0:128], [Rps[k % 2]], [RgB])

    def phase_merge(l, src):
        P.barrier()
        m0 = A.mark()
        gB = A.alloc([D], F32)
        RgB = Reg("gB")
        build_gateB(l, 2, gB, RgB)
        wo = A.alloc([8, D], BF16)
        Rwo = Reg("wo")
        dma("pool", wo[:, :, 0:512], kmajor(w_out[l], 0, 512), [], [Rwo])
        dma("pool", wo[:, :, 512:1024], kmajor(w_out[l], 512, 512), [], [Rwo])
        wm = [A.alloc([16, 256], BF16) for _ in range(2)]
        Rwm = [Reg("wm%d" % i) for i in range(2)]
        wg = [A.alloc([8, 3, 256], BF16) for _ in range(2)]
        Rwg = [Reg("wg%d" % i) for i in range(2)]
        mT = A.alloc([8, 1024], BF16)
        RmT = Reg("mT")
        gs = [A.alloc([512], BF16) for _ in range(6)]
        Rgs = [Reg("gs%d" % i) for i in range(6)]
        mm_ = [A.alloc([512], F32) for _ in range(4)]
        Rmm = [Reg("mm%d" % i) for i in range(4)]
        rot = {"b": 0, "g": 0, "m": 0}
        xt = [A.alloc([D], F32) for _ in range(2)]
        Rxt = [Reg("xtm%d" % i) for i in range(2)]
        tmp = A.alloc([512], F32)
        Rtmp = Reg("tmpm")
        it = 0
        xit = 0
        for hf in range(2):
            t0 = hf * 1024
            for oc in range(8):
                if oc % 2 == 0:
                    wfull, Rw_ = wm[it % 2], Rwm[it % 2]
                    gfull, Rg_ = wg[it % 2], Rwg[it % 2]
                    it += 1
                    dma("pool", wfull[:, 0:8, :], kmajor(w_br_a[l], oc * 128, 256), [], [Rw_])
                    dma("pool", wfull[:, 8:12, :], kmajor(w_br_b[l], oc * 128, 256), [], [Rw_])
                    dma("pool", wfull[:, 12:16, :], kmajor(w_br_c[l], oc * 128, 256), [], [Rw_])
                    for br in range(3):
                        dma("pool", gfull[:, :, br, :], kmajor(w_in[l], 5120 + br * 1024 + oc * 128, 256), [], [Rg_])
                osl = slice((oc % 2) * 128, (oc % 2) * 128 + 128)
                w_ = wfull[:, :, osl]
                g_ = gfull[:, :, :, osl]
                for cc in range(2):
                    tk = slice(t0 + cc * 512, t0 + (cc + 1) * 512)
                    ci = t0 // 512 + cc
                    gb = []
                    for br in range(3):
                        bk = rot["b"] % 8
                        rot["b"] += 1
                        gb.append(bk)
                        for k in range(8):
                            mm(psb[bk][:, :], g_[:, k, br, :], hT[:, k, tk], k == 0, k == 7, [Rg_, RhT[ci]], [Rps[bk]])
                        gsb, Rgsb = gs[rot["g"] % 6], Rgs[rot["g"] % 6]
                        rot["g"] += 1
                        gb[-1] = (bk, gsb, Rgsb)
                        act(gsb, psb[bk][:, :], AF.Sigmoid, [Rps[bk], Rv], [Rgsb],
                            bias=vecs[:, l, V_GATEB + br * 8 + oc:V_GATEB + br * 8 + oc + 1])
                    ab = []
                    for (nk_, k0_, ysrc, Rys) in ((8, 0, yaT, RyaT[hf]), (4, 8, ybT, RybT), (4, 12, ycT, RycT)):
                        bk = rot["b"] % 8
                        rot["b"] += 1
                        ab.append(bk)
                        for k in range(nk_):
                            mm(psb[bk][:, :], w_[:, k0_ + k, :], ysrc[:, k, tk], k == 0, k == nk_ - 1, [Rw_, Rys], [Rps[bk]])
                    m0_, Rm0_ = mm_[rot["m"] % 4], Rmm[rot["m"] % 4]
                    m1_, Rm1_ = mm_[(rot["m"] + 1) % 4], Rmm[(rot["m"] + 1) % 4]
                    rot["m"] += 2
                    tt("dve", m0_, psb[ab[0]][:, :], gb[0][1], ALU.mult, [Rps[ab[0]], gb[0][2]], [Rm0_])
                    tt("dve", m1_, psb[ab[1]][:, :], gb[1][1], ALU.mult, [Rps[ab[1]], gb[1][2]], [Rm1_])
                    tt("dve", m0_, m0_, m1_, ALU.add, [Rm0_, Rm1_], [Rm0_])
                    tt("dve", m1_, psb[ab[2]][:, :], gb[2][1], ALU.mult, [Rps[ab[2]], gb[2][2]], [Rm1_])
                    tt("dve", mT[:, oc, cc * 512:(cc + 1) * 512], m0_, m1_, ALU.add, [Rm0_, Rm1_], [RmT])
            for il in range(8):
                i = hf * 8 + il
                xb_, Rxb_ = xt[xit % 2], Rxt[xit % 2]
                xit += 1
                dma("sp", xb_, src[i * 128:(i + 1) * 128, :], [Ry[i]], [Rxb_])
                for nh in range(2):
                    bank = rot["b"] % 8
                    rot["b"] += 1
                    for k in range(8):
                        mm(psb[bank][:, :], mT[:, k, il * 128:(il + 1) * 128], wo[:, k, nh * 512:(nh + 1) * 512], k == 0, k == 7,
                           [RmT, Rwo], [Rps[bank]])
                    tt("dve", tmp, psb[bank][:, :], gB[:, nh * 512:(nh + 1) * 512], ALU.mult, [Rps[bank], RgB], [Rtmp])
                    tt("dve", xb_[:, nh * 512:(nh + 1) * 512], xb_[:, nh * 512:(nh + 1) * 512], tmp, ALU.add, [Rxb_, Rtmp], [Rxb_])
                dma("sp", y_d[i * 128:(i + 1) * 128, :], xb_, [Rxb_], [Ry[i]])
        A.release(m0)

    def phase_ffn(l, experts):
        P.barrier()
        m0 = A.mark()
        gB = A.alloc([D], F32)
        RgB = Reg("gB2")
        build_gateB(l, 5, gB, RgB)
        moe = experts[0][3] is not None
        comb = None
        if moe:
            comb = A.alloc([16, 8], F32)
            Rcomb = Reg("comb")
            rwb = A.alloc([64], BF16)
            Rrwb = Reg("rwb")
            dma("pool", rwb, rw_d[:, :], [], [Rrwb])
            lg = A.alloc([8], F32)
            Rlg = Reg("lg")
            tp = A.alloc([8], F32)
            Rtp = Reg("tp")
            wts = A.alloc([4], F32)
            Rwts = Reg("wts")
            msk = A.alloc([8], F32)
            Rmsk = Reg("msk")
            rwv = rwb.rearrange("p (k e) -> p k e", k=8)
            for i in range(16):
                for k in range(8):
                    mm(psb[0][:, 0:8], hT[:, k, i * 128:(i + 1) * 128], rwv[:, k, :], k == 0, k == 7, [RhT[i // 4], Rrwb], [Rps[0]])
                tt("dve", lg, psb[0][:, 0:8], vecs[:, l, V_RB:V_RB + 8], ALU.add, [Rps[0], Rv], [Rlg])
                P.op("dve", lambda e: e.max(out=tp, in_=lg), r=[Rlg], w=[Rtp])
                tt("dve", wts[:, 0:1], tp[:, 0:1], tp[:, 1:2], ALU.subtract, [Rtp], [Rwts])
                act(wts[:, 1:2], wts[:, 0:1], AF.Sigmoid, [Rwts], [Rwts])
                ts("dve", wts[:, 2:3], wts[:, 1:2], -1.0, 1.0, ALU.mult, ALU.add, [Rwts], [Rwts])
                ts("dve", msk, lg, tp[:, 0:1], wts[:, 1:2], ALU.is_equal, ALU.mult, [Rlg, Rtp, Rwts], [Rmsk])
                ts("dve", comb[:, i, :], lg, tp[:, 1:2], wts[:, 2:3], ALU.is_equal, ALU.mult, [Rlg, Rtp, Rwts], [Rcomb])
                tt("dve", comb[:, i, :], comb[:, i, :], msk, ALU.add, [Rcomb, Rmsk], [Rcomb])
        HT = 1024
        uT = A.alloc([NFC, HT], BF16)
        RuT = [Reg("uT%d" % c) for c in range(2)]
        w13 = [A.alloc([2, 8, 256], BF16) for _ in range(2)]
        Rw13 = [Reg("w13_%d" % i) for i in range(2)]
        w2b = [A.alloc([NFC, 512], BF16) for _ in range(3)]
        Rw2b = [Reg("w2b%d" % i) for i in range(3)]
        sl_ = [A.alloc([512], F32) for _ in range(2)]
        Rsl = [Reg("silu%d" % i) for i in range(2)]
        tmp = [A.alloc([512], F32) for _ in range(2)]
        Rtmp = [Reg("tmpf%d" % i) for i in range(2)]
        ybuf = [A.alloc([512], F32) for _ in range(3)]
        Rybuf = [Reg("ybuf%d" % i) for i in range(3)]
        wit = 0
        w2it = 0
        pit = 0
        sit = 0
        tit = 0
        groups = [(c0, 256) for c0 in range(0, DFF, 256)]
        for hf in range(2):
            t0 = hf * HT
            for (w1, w3, w2, e) in experts:
                w2slots = []
                for gi, (c0, nc_) in enumerate(groups):
                    wb, Rwb = w13[wit % 2], Rw13[wit % 2]
                    wit += 1
                    dma("pool", wb[:, 0, :, 0:nc_], kmajor(w1, c0, nc_), [], [Rwb])
                    dma("pool", wb[:, 1, :, 0:nc_], kmajor(w3, c0, nc_), [], [Rwb])
                    if gi in (2, 5):
                        nh = 0 if gi == 2 else 1
                        w2t, Rw2t = w2b[w2it % 3], Rw2b[w2it % 3]
                        w2it += 1
                        src2 = w2[:, nh * 512:(nh + 1) * 512].rearrange("(f p) n -> p f n", p=128)
                        dma("pool", w2t[:, 0:11, :], src2[:, 0:11, :], [], [Rw2t])
                        dma("pool", w2t[:, 11:22, :], src2[:, 11:22, :], [], [Rw2t])
                        w2slots.append((w2t, Rw2t))
                    for fcl in range(nc_ // 128):
                        f = c0 // 128 + fcl
                        for cc in range(2):
                            b1 = (pit % 2) * 2
                            pit += 1
                            for g in range(2):
                                for k in range(8):
                                    mm(psb[b1 + g][:, :], wb[:, g, k, fcl * 128:(fcl + 1) * 128], hT[:, k, t0 + cc * 512:t0 + (cc + 1) * 512],
                                       k == 0, k == 7, [Rwb, RhT[t0 // 512 + cc]], [Rps[b1 + g]])
                            s_, Rs_ = sl_[sit % 2], Rsl[sit % 2]
                            sit += 1
                            act(s_, psb[b1][:, :], AF.Silu, [Rps[b1]], [Rs_])
                            tt("dve", uT[:, f, cc * 512:(cc + 1) * 512], psb[b1 + 1][:, :], s_, ALU.mult, [Rps[b1 + 1], Rs_], [RuT[cc]])
                for nh in range(2):
                    w2t, Rw2t = w2slots[nh]
                    for il in range(8):
                        i = hf * 8 + il
                        bank = 4 + (pit % 4)
                        pit += 1
                        for f in range(NFC):
                            mm(psb[bank][:, :], uT[:, f, il * 128:(il + 1) * 128], w2t[:, f, :], f == 0, f == NFC - 1,
                               [RuT[il // 4], Rw2t], [Rps[bank]])
                        t_, Rt_ = tmp[tit % 2], Rtmp[tit % 2]
                        tit += 1
                        if moe:
                            stt(t_, psb[bank][:, :], comb[:, i, e:e + 1], gB[:, nh * 512:(nh + 1) * 512], ALU.mult, ALU.mult,
                                [Rps[bank], Rcomb, RgB], [Rt_])
                        else:
                            tt("dve", t_, psb[bank][:, :], gB[:, nh * 512:(nh + 1) * 512], ALU.mult, [Rps[bank], RgB], [Rt_])
                        yb_, Ryb_ = ybuf[tit % 3], Rybuf[tit % 3]
                        ysl = y_d[i * 128:(i + 1) * 128, nh * 512:(nh + 1) * 512]
                        dma("sp", yb_, ysl, [Ry[i]], [Ryb_])
                        tt("dve", yb_, yb_, t_, ALU.add, [Ryb_, Rt_], [Ryb_])
                        dma("sp", ysl, yb_, [Ryb_], [Ry[i]])
        A.release(m0)

    def dump(name, ap, shape, dt, regs):
        d = dbg_out(name, shape, dt)
        P.barrier()
        dma("sp", d, ap, regs, [])

    for l in range(2):
        src = x_d if l == 0 else y_d
        phase_norm(l, 0, src)
        if stage_end("norm1_%d" % l):
            dump("hT", hT, [128, 8, S], BF16, RhT)
            break
        P.barrier()
        A.release(cs_mark)
        A.alloc([S], F32)
        A.alloc([S], F32)
        compute_rope()
        phase_diff(l)
        if stage_end("diff_%d" % l):
            dump("ybT", ybT, [128, 4, S], BF16, [RybT])
            break
        phase_moba(l)
        if stage_end("moba_%d" % l):
            dump("ycT", ycT, [128, 4, S], BF16, [RycT])
            break
        A.release(cs_mark)
        phase_rnn(l)
        if stage_end("rnn_%d" % l):
            dump("yaT", yaT, [128, 8, S], BF16, RyaT)
            break
        phase_merge(l, src)
        if stage_end("merge_%d" % l):
            break
        A.release(base_mark)
        phase_norm(l, 1, y_d)
        if stage_end("norm2_%d" % l):
            dump("hT", hT, [128, 8, S], BF16, RhT)
            break
        if l == 0:
            phase_ffn(l, [(ffn_w1[0], ffn_w3[0], ffn_w2[0], None)])
        else:
            phase_ffn(l, [(moe_w1[0, e], moe_w3[0, e], moe_w2[0, e], e) for e in range(8)])
        if stage_end("ffn_%d" % l):
            break
        A.release(base_mark)
    P.finish()
    return nc, es, P, A, dbg_outs


_CACHE = {}


def prep_inputs(inp, b):
    pos = np.ascontiguousarray(np.broadcast_to(np.asarray(inp["positions"])[b][None, :], (128, S))).astype(np.int32)
    oh = np.zeros((8, S), np.float32)
    for n in range(8):
        oh[n, n * 256:(n + 1) * 256] = 1.0
    rw = np.ascontiguousarray(np.asarray(inp["router_w"])[0].reshape(8, 128, 8).transpose(1, 0, 2).reshape(128, 64))
    return {
        "x": np.ascontiguousarray(np.asarray(inp["x"])[b]),
        "cT": colform(np.asarray(inp["c"])[b], 8),
        "pos": pos,
        "onehot": oh,
        "rw": rw,
    }


SHARED = ["ada_w", "w_in", "lru_wa", "lru_wx", "w_br_a", "w_br_b", "w_br_c", "w_out", "ffn_w1", "ffn_w3", "ffn_w2",
          "moe_w1", "moe_w3", "moe_w2"]


def kernel(**inputs):
    inp = {k: np.asarray(v) for k, v in inputs.items()}
    if "prog" not in _CACHE:
        _CACHE["prog"] = build_program()
    nc = _CACHE["prog"][0]
    consts = make_consts()
    vecs = make_vecs(inp)
    shared = {k: np.ascontiguousarray(inp[k], dtype=np.float32) for k in SHARED}
    in_maps = []
    for b in range(8):
        m = prep_inputs(inp, b)
        m["consts"] = consts
        m["vecs"] = vecs
        m.update(shared)
        in_maps.append(m)
    res = run_bass_kernel_spmd(nc, in_maps, core_ids=list(range(8)))
    out = np.stack([np.asarray(r["y"]) for r in res.results], axis=0).astype(np.float32)
    return out
```

```python
import math
import numpy as np
from contextlib import ExitStack
import concourse.bass as bass
import concourse.mybir as mybir
from concourse.bass_utils import run_bass_kernel_spmd

F32 = mybir.dt.float32
BF16 = mybir.dt.bfloat16
I32 = mybir.dt.int32
AF = mybir.ActivationFunctionType
ALU = mybir.AluOpType
AX = mybir.AxisListType

S = 2048
D = 1024
DFF = 2816
NFC = 22
EPS = 1e-6
BIG = 30000.0


class Reg:
    __slots__ = ("name", "w", "r")

    def __init__(self, name=""):
        self.name = name
        self.w = {}
        self.r = {}


class Op:
    __slots__ = ("eng", "fn", "r", "w", "dma", "waits", "tok", "signal", "val", "idx", "barrier")

    def __init__(self, eng, fn, r, w, dma, barrier=False):
        self.eng = eng
        self.fn = fn
        self.r = r
        self.w = w
        self.dma = dma
        self.waits = []
        self.tok = None
        self.signal = False
        self.val = 0
        self.barrier = barrier


class Prog:
    NDMA = 32

    def __init__(self, nc, es):
        self.nc = nc
        self.ops = []
        self.engs = {"pe": nc.tensor, "act": nc.scalar, "dve": nc.vector, "pool": nc.gpsimd, "sp": nc.sync}
        self.sems = {k: es.enter_context(nc.semaphore("s_" + k)) for k in self.engs}
        self.dsems = [es.enter_context(nc.semaphore("d%d" % i)) for i in range(self.NDMA)]

    def op(self, eng, fn, r=(), w=(), dma=False):
        self.ops.append(Op(eng, fn, list(r), list(w), dma))

    def barrier(self):
        for k in self.engs:
            self.ops.append(Op(k, None, [], [], False, barrier=True))

    def finish(self):
        ops = self.ops
        seen = {k: {} for k in self.engs}
        by_eng = {k: [] for k in self.engs}
        dval = [0] * self.NDMA
        half = self.NDMA // 2
        dnext = {"pool": 0, "other": 0}
        for op in ops:
            X = op.eng
            sx = seen[X]
            waits = op.waits

            def need(tok):
                if tok[0] == "e":
                    _, Y, idx = tok
                    if sx.get(Y, -1) >= idx:
                        return
                    sx[Y] = idx
                    by_eng[Y][idx].signal = True
                    waits.append(tok)
                else:
                    _, s, v = tok
                    if sx.get(("d", s), 0) >= v:
                        return
                    sx[("d", s)] = v
                    waits.append(tok)

            if op.barrier:
                for Y in self.engs:
                    if Y != X and by_eng[Y]:
                        j = len(by_eng[Y]) - 1
                        while j >= 0 and (by_eng[Y][j].dma or by_eng[Y][j].fn is None):
                            j -= 1
                        if j >= 0:
                            need(("e", Y, j))
                for s in range(self.NDMA):
                    if dval[s] > 0:
                        need(("d", s, dval[s]))
                continue
            for reg in op.r:
                for tok in reg.w.values():
                    need(tok)
            for reg in op.w:
                for tok in list(reg.w.values()) + list(reg.r.values()):
                    if tok[0] == "e" and tok[1] == X and not op.dma and X == "pe":
                        continue
                    need(tok)
            op.idx = len(by_eng[X])
            by_eng[X].append(op)
            if op.dma:
                if X == "pool":
                    s = dnext["pool"]
                    dnext["pool"] = (s + 1) % half
                else:
                    s = half + dnext["other"]
                    dnext["other"] = (dnext["other"] + 1) % half
                if dval[s] > 0:
                    need(("d", s, dval[s]))
                dval[s] += 16
                tok = ("d", s, dval[s])
                key = ("d", s)
            else:
                tok = ("e", X, op.idx)
                key = X
            op.tok = tok
            for reg in op.r:
                reg.r[key] = tok
            for reg in op.w:
                reg.w[key] = tok
        cnt = {k: 0 for k in self.engs}
        for op in ops:
            if op.barrier:
                continue
            if (not op.dma) and op.signal:
                cnt[op.eng] += 1
                op.val = cnt[op.eng]
        nwait = 0
        for op in ops:
            e = self.engs[op.eng]
            for tok in op.waits:
                nwait += 1
                if tok[0] == "e":
                    e.wait_ge(self.sems[tok[1]], by_eng[tok[1]][tok[2]].val)
                else:
                    e.wait_ge(self.dsems[tok[1]], tok[2])
            if op.barrier:
                continue
            ins = op.fn(e)
            if op.dma:
                ins.then_inc(self.dsems[op.tok[1]], 16)
            elif op.signal:
                ins.then_inc(self.sems[op.eng], 1)
        sp = self.engs["sp"]
        for s, v in enumerate(dval):
            if v > 0:
                sp.wait_ge(self.dsems[s], v)
        self.stats = (len(ops), nwait, dict(cnt))


class Arena:
    def __init__(self, ap, nwords):
        self.ap = ap
        self.n = nwords
        self.top = 0
        self.peak = 0

    def alloc(self, shape, dt):
        nel = 1
        for s in shape:
            nel *= s
        bpe = 4 if dt in (F32, I32) else 2
        words = (nel * bpe + 3) // 4
        words = (words + 1) // 2 * 2
        assert self.top + words <= self.n, ("arena overflow", self.top, words, self.n)
        v = self.ap[:, self.top:self.top + words]
        self.top += words
        self.peak = max(self.peak, self.top)
        if dt != F32:
            v = v.bitcast(dt)
        v = v[:, 0:nel]
        if len(shape) == 2:
            v = v.rearrange("p (a b) -> p a b", a=shape[0], b=shape[1])
        elif len(shape) == 3:
            v = v.rearrange("p (a b c) -> p a b c", a=shape[0], b=shape[1], c=shape[2])
        return v

    def mark(self):
        return self.top

    def release(self, m):
        self.top = m


C_ID = 0
C_RT = 128
C_OB = 256
C_TRI = 384
C_ONES = 512
C_INVF = 640
C_EPS = 641
C_ONE = 642
C_HALFM = 643
C_NEGM = 644
C_OWN = 708
C_NPI = 772
C_NEGA = 776
C_OWNA = 904
NCONST = 1032

V_G1 = 0
V_G2 = 8
V_ADAB = 16
V_GATEB = 64
V_CONVW = 88
V_CONVB = 120
V_BA = 128
V_BX = 136
V_LAM = 144
V_DQN = 152
V_DKN = 153
V_MQN = 154
V_MKN = 155
V_SUBLN = 156
V_LQK = 160
V_RB = 416
NV = 424


def make_consts():
    c = np.zeros((128, NCONST), np.float32)
    c[:, C_ID:C_ID + 128] = np.eye(128, dtype=np.float32)
    rt = np.zeros((128, 128), np.float32)
    for blk in (0, 64):
        for m in range(64):
            if m < 32:
                rt[blk + m + 32, blk + m] = -1.0
            else:
                rt[blk + m - 32, blk + m] = 1.0
    c[:, C_RT:C_RT + 128] = rt
    ob = np.zeros((128, 128), np.float32)
    ob[0:64, 0:64] = 1.0 / 64
    ob[64:128, 64:128] = 1.0 / 64
    c[:, C_OB:C_OB + 128] = ob
    k = np.arange(128)[:, None]
    q = np.arange(128)[None, :]
    c[:, C_TRI:C_TRI + 128] = (q >= k).astype(np.float32)
    c[:, C_ONES:C_ONES + 128] = 1.0
    inv = (1.0 / (np.float32(10000.0) ** (np.arange(0, 64, 2, dtype=np.float32) / np.float32(64)))).astype(np.float32)
    c[:, C_INVF] = inv[np.arange(128) % 32]
    c[:, C_EPS] = EPS
    c[:, C_ONE] = 1.0
    c[:, C_HALFM] = -0.5
    for cur in range(8):
        for n in range(8):
            c[:, C_NEGM + cur * 8 + n] = -1e30 if n >= cur else 0.0
            c[:, C_OWN + cur * 8 + n] = 1.0 if n == cur else 0.0
    for i in range(16):
        for n in range(8):
            c[:, C_NEGA + i * 8 + n] = -1e30 if n >= i // 2 else 0.0
            c[:, C_OWNA + i * 8 + n] = 1.0 if n == i // 2 else 0.0
    return c


def colform(v, nchunk):
    return np.ascontiguousarray(v.reshape(nchunk, 128).T)


def make_vecs(inp):
    out = np.zeros((2, 128, NV), np.float32)
    for l in range(2):
        o = out[l]
        o[:, V_G1:V_G1 + 8] = colform(inp["norm1_g"][l], 8)
        o[:, V_G2:V_G2 + 8] = colform(inp["norm2_g"][l], 8)
        o[:, V_ADAB:V_ADAB + 48] = colform(inp["ada_b"][l], 48)
        for br in range(3):
            o[:, V_GATEB + br * 8:V_GATEB + br * 8 + 8] = colform(inp["gate_b"][l, br], 8)
        for tap in range(4):
            o[:, V_CONVW + tap * 8:V_CONVW + tap * 8 + 8] = colform(inp["conv_w"][l, tap], 8)
        o[:, V_CONVB:V_CONVB + 8] = colform(inp["conv_b"][l], 8)
        o[:, V_BA:V_BA + 8] = colform(inp["lru_ba"][l], 8)
        o[:, V_BX:V_BX + 8] = colform(inp["lru_bx"][l], 8)
        o[:, V_LAM:V_LAM + 8] = colform(inp["lru_lambda"][l], 8)
        o[:, V_DQN] = np.tile(inp["diff_qn"][l], 2)
        o[:, V_DKN] = np.tile(inp["diff_kn"][l], 2)
        o[:, V_MQN] = np.tile(inp["moba_qn"][l], 2)
        o[:, V_MKN] = np.tile(inp["moba_kn"][l], 2)
        o[:, V_SUBLN] = inp["diff_subln"][l]
        for j, nm in enumerate(["diff_lq1", "diff_lk1", "diff_lq2", "diff_lk2"]):
            o[:, V_LQK + j * 64:V_LQK + j * 64 + 64] = np.broadcast_to(inp[nm][l][None, :], (128, 64))
        o[:, V_RB:V_RB + 8] = np.broadcast_to(inp["router_b"][0][None, :], (128, 8))
    return out


def build_program(stop=None, dbg=None, sub=99):
    nc = bass.Bass("TRN2", target_bir_lowering=False)
    es = ExitStack()
    P = Prog(nc, es)

    def din(name, shape, dt=F32):
        return nc.dram_tensor(name, list(shape), dt, kind="ExternalInput").ap()

    x_d = din("x", [S, D])
    cT_d = din("cT", [128, 8])
    pos_d = din("pos", [128, S], I32)
    consts_d = din("consts", [128, NCONST])
    onehot_d = din("onehot", [8, S])
    vecs_d = din("vecs", [2, 128, NV])
    rw_d = din("rw", [128, 64])
    ada_w = din("ada_w", [2, D, 6 * D])
    w_in = din("w_in", [2, D, 8192])
    lru_wa = din("lru_wa", [2, 16, 64, 64])
    lru_wx = din("lru_wx", [2, 16, 64, 64])
    w_br_a = din("w_br_a", [2, D, D])
    w_br_b = din("w_br_b", [2, 512, D])
    w_br_c = din("w_br_c", [2, 512, D])
    w_out = din("w_out", [2, D, D])
    ffn_w1 = din("ffn_w1", [1, D, DFF])
    ffn_w3 = din("ffn_w3", [1, D, DFF])
    ffn_w2 = din("ffn_w2", [1, DFF, D])
    moe_w1 = din("moe_w1", [1, 8, D, DFF])
    moe_w3 = din("moe_w3", [1, 8, D, DFF])
    moe_w2 = din("moe_w2", [1, 8, DFF, D])
    y_d = nc.dram_tensor("y", [S, D], F32, kind="ExternalOutput").ap()
    dbg_outs = {}

    def dbg_out(name, shape, dt=F32):
        t = nc.dram_tensor("dbg_" + name, list(shape), dt, kind="ExternalOutput").ap()
        dbg_outs[name] = t
        return t

    NW = 53000
    arena_t = es.enter_context(nc.sbuf_tensor("arena", [128, NW], F32))
    A = Arena(arena_t, NW)
    psb = [es.enter_context(nc.psum_tensor("ps%d" % i, [128, 512], F32)) for i in range(8)]
    Rps = [Reg("ps%d" % i) for i in range(8)]

    def mm(out, lhsT, rhs, start, stop, r, w, skip=False):
        if skip:
            P.op("pe", lambda e: e.matmul(out, lhsT=lhsT, rhs=rhs, start=start, stop=stop, skip_group_check=True), r=r, w=w)
        else:
            P.op("pe", lambda e: e.matmul(out, lhsT=lhsT, rhs=rhs, start=start, stop=stop), r=r, w=w)

    def tr(out, in_, ident, r, w):
        P.op("pe", lambda e: e.transpose(out, in_, ident), r=r, w=w)

    def act(out, in_, func, r, w, scale=1.0, bias=None, accum=None):
        def f(e):
            kw = {}
            if bias is not None:
                kw["bias"] = bias
            if accum is not None:
                kw["accum_out"] = accum
            return e.activation(out=out, in_=in_, func=func, scale=scale, **kw)
        P.op("act", f, r=r, w=w)

    def tt(eng, out, in0, in1, op, r, w):
        P.op(eng, lambda e: e.tensor_tensor(out=out, in0=in0, in1=in1, op=op), r=r, w=w)

    def ts(eng, out, in0, s1, s2, op0, op1, r, w, accum=None):
        def f(e):
            if op1 is None:
                return e.tensor_scalar(out=out, in0=in0, scalar1=s1, scalar2=None, op0=op0)
            if accum is not None:
                return e.tensor_scalar(out=out, in0=in0, scalar1=s1, scalar2=s2, op0=op0, op1=op1, accum_out=accum)
            return e.tensor_scalar(out=out, in0=in0, scalar1=s1, scalar2=s2, op0=op0, op1=op1)
        P.op(eng, f, r=r, w=w)

    def stt(out, in0, scalar, in1, op0, op1, r, w, accum=None):
        def f(e):
            if accum is not None:
                return e.scalar_tensor_tensor(out=out, in0=in0, scalar=scalar, in1=in1, op0=op0, op1=op1, accum_out=accum)
            return e.scalar_tensor_tensor(out=out, in0=in0, scalar=scalar, in1=in1, op0=op0, op1=op1)
        P.op("dve", f, r=r, w=w)

    def cp(eng, out, in_, r, w):
        P.op(eng, lambda e: e.tensor_copy(out=out, in_=in_), r=r, w=w)

    def memset(eng, ap, val, w):
        P.op(eng, lambda e: e.memset(ap, val), w=w)

    def dma(eng, out, in_, r, w, accum=False):
        if accum:
            P.op(eng, lambda e: e.dma_start(out=out, in_=in_, accum_op=ALU.add), r=r, w=w, dma=True)
        else:
            P.op(eng, lambda e: e.dma_start(out=out, in_=in_), r=r, w=w, dma=True)

    def kmajor(w2d, c0, ncols):
        return w2d[:, c0:c0 + ncols].rearrange("(k p) n -> p k n", p=128)

    cst = A.alloc([NCONST], F32)
    Rc = Reg("consts")
    vecs = A.alloc([2, NV], F32)
    Rv = Reg("vecs")
    cb16 = A.alloc([5, 128], BF16)
    Rcb = Reg("cb16")
    ident_b = cb16[:, 0, :]
    rt_b = cb16[:, 1, :]
    ob_b = cb16[:, 2, :]
    tri_b = cb16[:, 3, :]
    ones_b = cb16[:, 4, :]
    ident_f = cst[:, C_ID:C_ID + 128]
    ones_f = cst[:, C_ONES:C_ONES + 128]
    eps_c = cst[:, C_EPS:C_EPS + 1]
    one_c = cst[:, C_ONE:C_ONE + 1]
    cTb = A.alloc([8], BF16)
    RcT = Reg("cT")
    modc = A.alloc([2, 48], F32)
    Rmod = Reg("mod")
    AB = A.alloc([2, 4, 8], F32)
    RAB = Reg("AB")
    misc = A.alloc([2, 40], F32)
    Rmisc = Reg("misc")
    M_C1, M_C2, M_NLAM, M_SUBG, M_NBA, M_TMP = 0, 8, 16, 17, 18, 26
    hT = A.alloc([8, S], BF16)
    RhT = [Reg("hT%d" % c) for c in range(4)]
    Ry = [Reg("y%d" % i) for i in range(16)]
    base_mark = A.mark()
    yaT = A.alloc([8, S], BF16)
    RyaT = [Reg("yaT%d" % c) for c in range(2)]
    ybT = A.alloc([4, S], BF16)
    RybT = Reg("ybT")
    ycT = A.alloc([4, S], BF16)
    RycT = Reg("ycT")
    cs_mark = A.mark()
    cosT = A.alloc([S], F32)
    sinT = A.alloc([S], F32)
    Rcs = Reg("cossin")
    A.release(base_mark)

    dma("sp", cst, consts_d[:, :], [], [Rc])
    dma("sp", vecs, vecs_d.rearrange("l p v -> p l v"), [], [Rv])
    for j, c0 in enumerate([C_ID, C_RT, C_OB, C_TRI, C_ONES]):
        dma("pool", cb16[:, j, :], consts_d[:, c0:c0 + 128], [], [Rcb])
    dma("pool", cTb, cT_d[:, :], [], [RcT])

    def compute_rope():
        m0 = A.mark()
        posi = A.alloc([S], I32)
        Rpos = Reg("pos")
        ang = A.alloc([S], F32)
        Rang = Reg("ang")
        kk = A.alloc([S], F32)
        Rkk = Reg("kk")
        rr = A.alloc([S], F32)
        Rrr = Reg("rr")
        dma("sp", posi, pos_d[:, :], [], [Rpos])
        cp("dve", ang, posi, [Rpos], [Rang])
        ts("dve", ang, ang, cst[:, C_INVF:C_INVF + 1], None, ALU.mult, None, [Rang, Rc], [Rang])
        MAGIC = 12582912.0
        C1 = 6.28125
        C2 = 2.0 * math.pi - 6.28125
        for which, dst in ((0, sinT), (1, cosT)):
            src = ang
            if which == 1:
                ts("dve", rr, ang, math.pi / 2, None, ALU.add, None, [Rang], [Rrr])
                src = rr
            ts("dve", kk, src, 1.0 / (2.0 * math.pi), MAGIC, ALU.mult, ALU.add, [Rang, Rrr], [Rkk])
            ts("dve", kk, kk, MAGIC, None, ALU.subtract, None, [Rkk], [Rkk])
            stt(rr, kk, -C1, src, ALU.mult, ALU.add, [Rkk, Rang, Rrr], [Rrr])
            stt(rr, kk, -C2, rr, ALU.mult, ALU.add, [Rkk, Rrr], [Rrr])
            ts("dve", rr, rr, 3.1415925, -3.1415925, ALU.min, ALU.max, [Rrr], [Rrr])
            act(dst, rr, AF.Sin, [Rrr], [Rcs])
        A.release(m0)

    m0 = A.mark()
    adaw = [A.alloc([8, 512], BF16) for _ in range(2)]
    Radaw = [Reg("adaw%d" % i) for i in range(2)]
    it = 0
    for l in range(2):
        for g in range(12):
            buf, Rb = adaw[it % 2], Radaw[it % 2]
            it += 1
            dma("pool", buf, kmajor(ada_w[l], g * 512, 512), [], [Rb])
            for cc in range(4):
                col = g * 4 + cc
                for k in range(8):
                    mm(psb[0][:, l * 48 + col:l * 48 + col + 1], buf[:, k, cc * 128:(cc + 1) * 128], cTb[:, k:k + 1],
                       k == 0, k == 7, [Rb, RcT], [Rps[0]], skip=True)
    for l in range(2):
        tt("dve", modc[:, l, :], psb[0][:, l * 48:(l + 1) * 48], vecs[:, l, V_ADAB:V_ADAB + 48], ALU.add,
           [Rps[0], Rv], [Rmod])
        for wh, (gcol, scq, shq) in enumerate(((V_G1, 1, 0), (V_G2, 4, 3))):
            stt(AB[:, l, 2 * wh, :], modc[:, l, scq * 8:scq * 8 + 8], 1.0, vecs[:, l, gcol:gcol + 8], ALU.add, ALU.mult,
                [Rmod, Rv], [RAB])
            cp("dve", AB[:, l, 2 * wh + 1, :], modc[:, l, shq * 8:shq * 8 + 8], [Rmod], [RAB])
    A.release(m0)

    for l in range(2):
        mt = misc[:, l, M_TMP:M_TMP + 8]
        act(mt, vecs[:, l, V_LAM:V_LAM + 8], AF.Exp, [Rv], [Rmisc], scale=-1.0)
        act(mt, mt, AF.Ln, [Rmisc, Rc], [Rmisc], bias=one_c)
        ts("dve", misc[:, l, M_C1:M_C1 + 8], mt, -8.0, None, ALU.mult, None, [Rmisc], [Rmisc])
        ts("dve", misc[:, l, M_C2:M_C2 + 8], mt, -16.0, None, ALU.mult, None, [Rmisc], [Rmisc])
        lam_init = 0.8 - 0.6 * math.exp(-0.3 * l)
        pr = misc[:, l, M_TMP + 8:M_TMP + 10]
        junk = A.alloc([64], F32)
        for j in range(2):
            stt(junk, vecs[:, l, V_LQK + (2 * j) * 64:V_LQK + (2 * j) * 64 + 64], 1.0,
                vecs[:, l, V_LQK + (2 * j + 1) * 64:V_LQK + (2 * j + 1) * 64 + 64], ALU.mult, ALU.mult,
                [Rv], [Rmisc], accum=pr[:, j:j + 1])
        act(pr, pr, AF.Exp, [Rmisc], [Rmisc])
        tt("dve", misc[:, l, M_NLAM:M_NLAM + 1], pr[:, 1:2], pr[:, 0:1], ALU.subtract, [Rmisc], [Rmisc])
        ts("dve", misc[:, l, M_NLAM:M_NLAM + 1], misc[:, l, M_NLAM:M_NLAM + 1], -lam_init, None, ALU.add, None,
           [Rmisc], [Rmisc])
        ts("dve", misc[:, l, M_SUBG:M_SUBG + 1], vecs[:, l, V_SUBLN:V_SUBLN + 1], 1.0 - lam_init, None, ALU.mult, None,
           [Rv], [Rmisc])
    A.release(base_mark)

    state = {"done": False}

    def stage_end(name):
        if stop == name:
            state["done"] = True
        return state["done"]

    def phase_norm(l, wh, src):
        P.barrier()
        m0 = A.mark()
        NB_ = 4
        xt = [A.alloc([D], F32) for _ in range(NB_)]
        Rxt = [Reg("xt%d" % i) for i in range(NB_)]
        xn = [A.alloc([D], BF16) for _ in range(NB_)]
        Rxn = [Reg("xn%d" % i) for i in range(NB_)]
        junk = A.alloc([D], BF16)
        Rjunk = Reg("junk")
        st = A.alloc([16, 4], F32)
        Rsts = [Reg("st%d" % i) for i in range(16)]
        for i in range(16):
            b = i % NB_
            c = i // 4
            Rst = Rsts[i]
            dma("sp", xt[b], src[i * 128:(i + 1) * 128, :], [Ry[i]], [Rxt[b]])
            stt(junk, xt[b], 1.0, xt[b], ALU.mult, ALU.mult, [Rxt[b]], [Rjunk, Rst], accum=st[:, i, 0:1])
            act(st[:, i, 1:2], st[:, i, 0:1], AF.Ln, [Rst, Rc], [Rst], scale=1.0 / D, bias=eps_c)
            act(st[:, i, 2:3], st[:, i, 1:2], AF.Exp, [Rst], [Rst], scale=-0.5)
            act(xn[b], xt[b], AF.Identity, [Rxt[b], Rst], [Rxn[b]], scale=st[:, i, 2:3])
            pset = (c % 2) * 4
            for k in range(8):
                bank = pset + k // 2
                pv = psb[bank][:, :].bitcast(BF16)
                off = (k % 2) * 512 + (i % 4) * 128
                tr(pv[:, off:off + 128], xn[b][:, k * 128:(k + 1) * 128], ident_b, [Rxn[b], Rcb], [Rps[bank]])
            if i % 4 == 3:
                for k in range(8):
                    bank = pset + k // 2
                    pv = psb[bank][:, :].bitcast(BF16)
                    off = (k % 2) * 512
                    o = hT[:, k, c * 512:(c + 1) * 512]
                    a_col = AB[:, l, 2 * wh, k:k + 1]
                    b_col = AB[:, l, 2 * wh + 1, k:k + 1]
                    if k % 2 == 0:
                        act(o, pv[:, off:off + 512], AF.Identity, [Rps[bank], RAB], [RhT[c]], scale=a_col, bias=b_col)
                    else:
                        ts("dve", o, pv[:, off:off + 512], a_col, b_col, ALU.mult, ALU.add, [Rps[bank], RAB], [RhT[c]])
        A.release(m0)

    def phase_rnn(l):
        P.barrier()
        m0 = A.mark()
        H = 1024
        LW = A.alloc([16, 128], BF16)
        RLW = Reg("LW")
        memset("pool", LW, 0.0, [RLW])
        for g, src in enumerate((lru_wa, lru_wx)):
            sv = src[l].rearrange("(j h) d e -> h d j e", h=2)
            for hb in range(2):
                dma("pool", LW[hb * 64:(hb + 1) * 64, g * 8:(g + 1) * 8, hb * 64:(hb + 1) * 64], sv[hb], [], [RLW])
        wxg = [A.alloc([2, 8, 128], BF16) for _ in range(8)]
        Rwxg = [Reg("wxg%d" % i) for i in range(8)]
        for j in range(8):
            dma("pool", wxg[j][:, 0, :, :], kmajor(w_in[l], j * 128, 128), [], [Rwxg[j]])
            dma("pool", wxg[j][:, 1, :, :], kmajor(w_in[l], 1024 + j * 128, 128), [], [Rwxg[j]])
        xr = [A.alloc([H + 4], F32) for _ in range(2)]
        Rxr = [Reg("xr%d" % i) for i in range(2)]
        hh = [A.alloc([H], F32) for _ in range(2)]
        Rhh = [Reg("hh%d" % i) for i in range(2)]
        names = ["xc", "r", "ig", "g", "tg", "sg", "a", "a2", "u"]
        B = {n: A.alloc([H], F32) for n in names}
        R = {n: Reg(n) for n in names}
        xcb = A.alloc([H], BF16)
        Rxcb = Reg("xcb")
        vl = lambda c0, j: vecs[:, l, c0 + j:c0 + j + 1]
        it = 0
        for j in range(8):
            wb, Rwb = wxg[j], Rwxg[j]
            for hf in range(2):
                t0 = hf * H
                xb_, Rxb_ = xr[it % 2], Rxr[it % 2]
                xp_, Rxp_ = xr[(it + 1) % 2], Rxr[(it + 1) % 2]
                hb_, Rhb_ = hh[it % 2], Rhh[it % 2]
                hp_, Rhp_ = hh[(it + 1) % 2], Rhh[(it + 1) % 2]
                it += 1
                for g in range(2):
                    for cc in range(2):
                        bank = g * 2 + cc
                        for k in range(8):
                            mm(psb[bank][:, :], wb[:, g, k, :], hT[:, k, t0 + cc * 512:t0 + (cc + 1) * 512], k == 0, k == 7,
                               [Rwb, RhT[(t0 // 512) + cc]], [Rps[bank]])
                if hf == 0:
                    memset("pool", xb_[:, 0:3], 0.0, [Rxb_])
                else:
                    cp("pool", xb_[:, 0:3], xp_[:, H:H + 3], [Rxp_], [Rxb_])
                for cc in range(2):
                    act(xb_[:, 3 + cc * 512:3 + (cc + 1) * 512], psb[cc][:, :], AF.Identity, [Rps[cc]], [Rxb_])
                    act(B["g"][:, cc * 512:(cc + 1) * 512], psb[2 + cc][:, :], AF.Identity, [Rps[2 + cc]], [R["g"]])
                act(B["xc"], xb_[:, 3:3 + H], AF.Identity, [Rxb_, Rv], [R["xc"]], scale=vl(V_CONVW + 3 * 8, j), bias=vl(V_CONVB, j))
                for tap in (2, 1, 0):
                    stt(B["xc"], xb_[:, tap:tap + H], vl(V_CONVW + tap * 8, j), B["xc"], ALU.mult, ALU.add,
                        [Rxb_, R["xc"], Rv], [R["xc"]])
                cp("pool", xcb, B["xc"], [R["xc"]], [Rxcb])
                for g in range(2):
                    for cc in range(2):
                        bank = 4 + g * 2 + cc
                        mm(psb[bank][:, :], LW[:, g * 8 + j, :], xcb[:, cc * 512:(cc + 1) * 512], True, True, [RLW, Rxcb], [Rps[bank]])
                for cc in range(2):
                    sl = slice(cc * 512, (cc + 1) * 512)
                    act(B["r"][:, sl], psb[4 + cc][:, :], AF.Sigmoid, [Rps[4 + cc], Rv], [R["r"]], bias=vl(V_BA, j))
                    act(B["ig"][:, sl], psb[6 + cc][:, :], AF.Sigmoid, [Rps[6 + cc], Rv], [R["ig"]], bias=vl(V_BX, j))
                tt("pool", B["tg"], B["g"], B["g"], ALU.mult, [R["g"]], [R["tg"]])
                ts("pool", B["tg"], B["tg"], 0.044715, 1.0, ALU.mult, ALU.add, [R["tg"]], [R["tg"]])
                tt("pool", B["tg"], B["tg"], B["g"], ALU.mult, [R["tg"], R["g"]], [R["tg"]])
                act(B["sg"], B["tg"], AF.Sigmoid, [R["tg"]], [R["sg"]], scale=1.5957691216057308)
                act(B["a"], B["r"], AF.Exp, [R["r"], Rmisc], [R["a"]], scale=misc[:, l, M_C1 + j:M_C1 + j + 1])
                act(B["a2"], B["r"], AF.Exp, [R["r"], Rmisc], [R["a2"]], scale=misc[:, l, M_C2 + j:M_C2 + j + 1])
                act(B["a2"], B["a2"], AF.Sqrt, [R["a2"], Rc], [R["a2"]], scale=-1.0, bias=one_c)
                tt("dve", B["u"], B["a2"], B["ig"], ALU.mult, [R["a2"], R["ig"]], [R["u"]])
                tt("dve", B["u"], B["u"], B["xc"], ALU.mult, [R["u"], R["xc"]], [R["u"]])
                if hf == 0:
                    P.op("dve", lambda e, o=hb_, a=B["a"], u=B["u"]: e.tensor_tensor_scan(out=o, data0=a, data1=u, initial=0.0, op0=ALU.mult, op1=ALU.add),
                         r=[R["a"], R["u"]], w=[Rhb_])
                else:
                    P.op("dve", lambda e, o=hb_, a=B["a"], u=B["u"], ini=hp_[:, H - 1:H]: e.tensor_tensor_scan(out=o, data0=a, data1=u, initial=ini, op0=ALU.mult, op1=ALU.add),
                         r=[R["a"], R["u"], Rhp_], w=[Rhb_])
                tt("pool", B["sg"], B["sg"], B["g"], ALU.mult, [R["sg"], R["g"]], [R["sg"]])
                tt("dve", yaT[:, j, t0:t0 + H], B["sg"], hb_, ALU.mult, [R["sg"], Rhb_], [RyaT[hf]])
        A.release(m0)

    def seq_gen(gens):
        for g in gens:
            for _ in g:
                yield

    def spaced(g, n):
        for _ in g:
            yield
            for _i in range(n):
                yield

    def qk_gen(l, wt, Rwt, gcol, outs, W, pb, sbk):
        for c in range(4):
            tk = slice(c * 512, (c + 1) * 512)
            for k in range(8):
                mm(psb[pb][:, :], wt[:, k, :], hT[:, k, tk], k == 0, k == 7, [Rwt, RhT[c]], [Rps[pb]])
            yield
            act(W["sqb"], psb[pb][:, :], AF.Square, [Rps[pb]], [W["Rsqb"]])
            yield
            mm(psb[sbk][:, :], ob_b, W["sqb"], True, True, [Rcb, W["Rsqb"]], [Rps[sbk]])
            yield
            act(W["rstd"], psb[sbk][:, :], AF.Ln, [Rps[sbk], Rc], [W["Rrstd"]], bias=eps_c)
            act(W["rstd"], W["rstd"], AF.Exp, [W["Rrstd"]], [W["Rrstd"]], scale=-0.5)
            yield
            stt(W["qn"], psb[pb][:, :], vecs[:, l, gcol:gcol + 1], W["rstd"], ALU.mult, ALU.mult,
                [Rps[pb], Rv, W["Rrstd"]], [W["Rqn"]])
            yield
            act(W["qnb"], W["qn"], AF.Identity, [W["Rqn"]], [W["Rqnb"]])
            yield
            mm(psb[sbk][:, :], rt_b, W["qnb"], True, True, [Rcb, W["Rqnb"]], [Rps[sbk]])
            yield
            tt("dve", W["t2"], psb[sbk][:, :], sinT[:, tk], ALU.mult, [Rps[sbk], Rcs], [W["Rt2"]])
            tt("dve", W["qn"], W["qn"], cosT[:, tk], ALU.mult, [W["Rqn"], Rcs], [W["Rqn"]])
            yield
            for (o_ap, ps_, Ro) in outs(c):
                tt("dve", o_ap, W["qn"][ps_, :], W["t2"][ps_, :], ALU.add, [W["Rqn"], W["Rt2"]], [Ro])
            yield

    def drive(gens):
        gens = list(gens)
        while gens:
            for g in list(gens):
                try:
                    next(g)
                except StopIteration:
                    gens.remove(g)

    def qk_work(tag=""):
        H = 512
        W = {}
        W["sqb"] = A.alloc([H], BF16)
        W["rstd"] = A.alloc([H], F32)
        W["qn"] = A.alloc([H], F32)
        W["qnb"] = A.alloc([H], BF16)
        W["t2"] = A.alloc([H], F32)
        for n in ("sqb", "rstd", "qn", "qnb", "t2"):
            W["R" + n] = Reg(n + tag)
        return W

    def run_pipeline(tiles, S_step, AV_step, Dp, bg=None):
        deferred = []
        n = len(tiles)
        for idx in range(n + Dp):
            if bg is not None:
                next(bg, None)
            if idx < n:
                S_step(idx, tiles[idx])
            if idx >= Dp:
                more = AV_step(idx - Dp, tiles[idx - Dp])
                for (dl, fn) in (more or []):
                    deferred.append((idx + dl, fn))
            due = [f for (t_, f) in deferred if t_ <= idx]
            deferred = [(t_, f) for (t_, f) in deferred if t_ > idx]
            for f in due:
                f()
        for (t_, f) in deferred:
            f()

    def phase_diff(l):
        P.barrier()
        m0 = A.mark()
        W = qk_work()
        W2 = qk_work("k")
        wv = A.alloc([8, 512], BF16)
        Rwv = Reg("wv")
        Vd = A.alloc([16, 4, 130], BF16)
        RVd = Reg("Vd")
        wqk4 = [[A.alloc([8, 128], BF16) for _ in range(2)] for _ in range(2)]
        Rwqk4 = [[Reg("wqk%d_%d" % (i, j_)) for j_ in range(2)] for i in range(2)]
        qT2 = [A.alloc([S], BF16) for _ in range(2)]
        kT2 = [A.alloc([S], BF16) for _ in range(2)]
        RqT2 = [Reg("qT%d" % i) for i in range(2)]
        RkT2 = [Reg("kT%d" % i) for i in range(2)]
        Eb = [A.alloc([256], BF16) for _ in range(3)]
        REb = [Reg("E%d" % i) for i in range(3)]
        Osb = A.alloc([2, 2, 130], F32)
        ROsb = Reg("Osb")
        sm = A.alloc([16], F32)
        Rsm = Reg("sm")
        o0 = A.alloc([128], F32)
        Ro0 = Reg("o0")
        junk = A.alloc([128], F32)
        Rjunk = Reg("junkd")
        onb = A.alloc([128], BF16)
        Ronb = Reg("onb")
        dma("pool", wv, kmajor(w_in[l], 3072, 512), [], [Rwv])
        memset("pool", Vd[:, :, :, 128:129], 1.0, [RVd])
        for i in range(16):
            bank = i % 2
            for k in range(8):
                mm(psb[bank][:, :], hT[:, k, i * 128:(i + 1) * 128], wv[:, k, :], k == 0, k == 7, [RhT[i // 4], Rwv], [Rps[bank]])
            act(Vd[:, i, :, 0:128], psb[bank][:, :].rearrange("p (h d) -> p h d", h=4), AF.Identity, [Rps[bank]], [RVd])
        NS, NE = 4, 4
        RS = [Reg("Sd%d" % i) for i in range(NS)]
        onbs = [A.alloc([128], BF16) for _ in range(6)]
        Ronbs = [Reg("onb%d" % i) for i in range(6)]
        Osb2 = [Osb, A.alloc([2, 2, 130], F32)]
        ROsb2 = [ROsb, Reg("Osb1")]
        sm2 = [sm, A.alloc([16], F32)]
        Rsm2 = [Rsm, Reg("sm1")]
        Eb4 = Eb + [A.alloc([256], BF16)]
        REb4 = REb + [Reg("E3")]
        cnt = {"onb": 0, "pv": 0}
        RpT = [Reg("pT0"), Reg("pT1")]

        def prep_gens(hn, banks_q, banks_k):
            pb_ = hn % 2
            wq_, wk_ = wqk4[pb_]
            Rwq_, Rwk_ = Rwqk4[pb_]
            dma("pool", wq_, kmajor(w_in[l], 2048 + hn * 128, 128), [], [Rwq_])
            dma("pool", wk_, kmajor(w_in[l], 2560 + hn * 128, 128), [], [Rwk_])
            qd, kd, Rqd, Rkd = qT2[pb_], kT2[pb_], RqT2[pb_], RkT2[pb_]
            gq = qk_gen(l, wq_, Rwq_, V_DQN, lambda c: [(qd[:, c * 512:(c + 1) * 512], slice(0, 128), Rqd)], W, banks_q[0], banks_q[1])
            gk = qk_gen(l, wk_, Rwk_, V_DKN, lambda c: [(kd[:, c * 512:(c + 1) * 512], slice(0, 128), Rkd)], W2, banks_k[0], banks_k[1])
            return gq, gk

        drive(prep_gens(0, (0, 2), (1, 3)))
        for h in range(4):
            qT, kT, RqT, RkT = qT2[h % 2], kT2[h % 2], RqT2[h % 2], RkT2[h % 2]
            bg = None
            if h < 3:
                gq_, gk_ = prep_gens(h + 1, (0, 1), (0, 1))
                bg = spaced(seq_gen([gq_, gk_]), 1)
            tiles = []
            for qc in range(8):
                for j in range(2):
                    nk = 2 * qc + 2
                    for kt in range(nk):
                        tiles.append((qc, j, kt, kt == nk - 1))

            def S_step(idx, t, qT=qT, kT=kT, RqT=RqT, RkT=RkT):
                qc, j, kt, lastk = t
                q0 = qc * 256
                k0 = kt * 128
                lo = max(q0, k0)
                ncols = q0 + 256 - lo
                sbank = (4, 5, 2, 3)[idx % NS]
                sps = psb[sbank][:, 0:ncols]
                E, RE = Eb4[idx % NE], REb4[idx % NE]
                rows = slice(64 * j, 64 * j + 64)
                mm(sps, kT[rows, k0:k0 + 128], qT[rows, lo:q0 + 256], True, True, [RkT, RqT], [Rps[sbank]])
                act(E[:, 0:ncols], sps, AF.Exp, [Rps[sbank]], [RE], scale=0.125)
                if k0 >= q0:
                    tt("dve", E[:, 0:128], E[:, 0:128], tri_b, ALU.mult, [RE, Rcb], [RE])

            def AV_step(idx, t, h=h):
                qc, j, kt, lastk = t
                q0 = qc * 256
                k0 = kt * 128
                lo = max(q0, k0)
                E, RE = Eb4[idx % NE], REb4[idx % NE]
                obank = 6 + j
                O_, RO_ = Osb2[qc % 2], ROsb2[qc % 2]
                sm_, Rsm_ = sm2[qc % 2], Rsm2[qc % 2]
                for qt in range(2):
                    qs = q0 + qt * 128
                    if qs < k0:
                        continue
                    off = qs - lo
                    ov = psb[obank][:, qt * 130:qt * 130 + 129]
                    mm(ov, E[:, off:off + 128], Vd[:, kt, h, 0:129], (kt == 0 and qt == 0), kt == (qs // 128),
                       [RE, RVd], [Rps[obank]], skip=True)
                if not lastk:
                    return None
                act(O_[:, j, :, 0:129], psb[obank][:, 0:260].rearrange("p (a b) -> p a b", a=2)[:, :, 0:129], AF.Identity,
                    [Rps[obank]], [RO_])
                if j == 0:
                    return None
                P.op("dve", lambda e, o=sm_[:, 0:4], i=O_[:, :, :, 128]: e.reciprocal(out=o.rearrange("p (a b) -> p a b", a=2), in_=i),
                     r=[RO_], w=[Rsm_])
                ts("dve", sm_[:, 4:6], sm_[:, 2:4], misc[:, l, M_NLAM:M_NLAM + 1], None, ALU.mult, None, [Rsm_, Rmisc], [Rsm_])
                used = []
                for qt in range(2):
                    ob_, Rob_ = onbs[cnt["onb"] % 6], Ronbs[cnt["onb"] % 6]
                    cnt["onb"] += 1
                    used.append((ob_, Rob_))
                    ts("dve", o0, O_[:, 0, qt, 0:128], sm_[:, qt:qt + 1], None, ALU.mult, None, [RO_, Rsm_], [Ro0])
                    stt(o0, O_[:, 1, qt, 0:128], sm_[:, 4 + qt:5 + qt], o0, ALU.mult, ALU.add, [RO_, Rsm_, Ro0], [Ro0])
                    stt(junk, o0, 1.0, o0, ALU.mult, ALU.mult, [Ro0], [Rjunk, Rsm_], accum=sm_[:, 8 + qt:9 + qt])
                    act(sm_[:, 10 + qt:11 + qt], sm_[:, 8 + qt:9 + qt], AF.Ln, [Rjunk, Rsm_, Rc], [Rsm_], scale=1.0 / 128, bias=eps_c)
                    act(sm_[:, 12 + qt:13 + qt], sm_[:, 10 + qt:11 + qt], AF.Exp, [Rsm_], [Rsm_], scale=-0.5)
                    ts("dve", ob_, o0, sm_[:, 12 + qt:13 + qt], None, ALU.mult, None, [Ro0, Rsm_], [Rob_])
                pvi = cnt["pv"] % 2
                cnt["pv"] += 1

                def fin(used=used, pvi=pvi, q0=q0, h=h):
                    pv = psb[1][:, :].bitcast(BF16)
                    for qt in range(2):
                        tr(pv[:, pvi * 256 + qt * 128:pvi * 256 + (qt + 1) * 128], used[qt][0], ident_b, [used[qt][1], Rcb], [Rps[1]])
                    act(ybT[:, h, q0:q0 + 256], pv[:, pvi * 256:(pvi + 1) * 256], AF.Identity, [Rps[1], Rmisc], [RybT],
                        scale=misc[:, l, M_SUBG:M_SUBG + 1])
                return [(4, fin)]

            run_pipeline(tiles, S_step, AV_step, 2, bg=bg)
            if bg is not None:
                for _ in bg:
                    pass
        A.release(m0)

    def phase_moba(l):
        P.barrier()
        m0 = A.mark()
        W = qk_work()
        W2 = qk_work("k")
        wv = A.alloc([8, 512], BF16)
        Rwv = Reg("wvm")
        Vm = A.alloc([16, 8, 66], BF16)
        RVm = Reg("Vm")
        wqk = [A.alloc([8, 128], BF16) for _ in range(2)]
        Rwqk = [Reg("wqkm%d" % i) for i in range(2)]
        Qe = A.alloc([S], BF16)
        Qo = A.alloc([S], BF16)
        Ke = A.alloc([S], BF16)
        Ko = A.alloc([S], BF16)
        RQe, RQo, RKe, RKo = Reg("Qe"), Reg("Qo"), Reg("Ke"), Reg("Ko")
        Eb = [A.alloc([512], BF16) for _ in range(4)]
        REb = [Reg("Em%d" % i) for i in range(4)]
        km = A.alloc([16], F32)
        Rkm = Reg("km")
        kmb = A.alloc([16], BF16)
        Rkmb = Reg("kmb")
        gmA = A.alloc([2, 16, 8], F32)
        Rgm = Reg("gm")
        topA = A.alloc([2, 16, 8], F32)
        Rtop = Reg("top")
        biasA = A.alloc([16, 128], BF16)
        Rbias = Reg("biasA")
        memset("pool", biasA, 0.0, [Rbias])
        yct = A.alloc([16, 128], BF16)
        Ryct = Reg("yct")
        rden2 = [A.alloc([4], F32) for _ in range(2)]
        Rrden2 = [Reg("rden%d" % i) for i in range(2)]
        memset("pool", Qo[0:64, :], 0.0, [RQo])
        memset("pool", Ko[0:64, :], 0.0, [RKo])
        memset("pool", Ke[64:128, :], 0.0, [RKe])
        memset("pool", Qe[64:128, :], 0.0, [RQe])
        dma("pool", Ke[64:72, :], onehot_d[:, :], [], [RKe])
        dma("pool", Ko[0:8, :], onehot_d[:, :], [], [RKo])
        dma("pool", wv, kmajor(w_in[l], 4608, 512), [], [Rwv])
        memset("pool", Vm[:, :, :, 64:65], 1.0, [RVm])
        for i in range(16):
            bank = i % 2
            for k in range(8):
                mm(psb[bank][:, :], hT[:, k, i * 128:(i + 1) * 128], wv[:, k, :], k == 0, k == 7, [RhT[i // 4], Rwv], [Rps[bank]])
            act(Vm[:, i, :, 0:64], psb[bank][:, :].rearrange("p (h d) -> p h d", h=8), AF.Identity, [Rps[bank]], [RVm])
        eit = 0
        if sub <= 0:
            A.release(m0)
            return
        for cpi in range(4):
            dma("pool", wqk[0], kmajor(w_in[l], 3584 + cpi * 128, 128), [], [Rwqk[0]])
            dma("pool", wqk[1], kmajor(w_in[l], 4096 + cpi * 128, 128), [], [Rwqk[1]])
            drive([qk_gen(l, wqk[0], Rwqk[0], V_MQN,
                          lambda c: [(Qe[0:64, c * 512:(c + 1) * 512], slice(0, 64), RQe),
                                     (Qo[64:128, c * 512:(c + 1) * 512], slice(64, 128), RQo)], W, 0, 2),
                   qk_gen(l, wqk[1], Rwqk[1], V_MKN,
                          lambda c: [(Ke[0:64, c * 512:(c + 1) * 512], slice(0, 64), RKe),
                                     (Ko[64:128, c * 512:(c + 1) * 512], slice(64, 128), RKo)], W2, 1, 3)])
            if sub <= 1:
                continue
            P.op("dve", lambda e: e.tensor_reduce(out=km[0:64, 0:8], in_=Ke[0:64, :].rearrange("p (n t) -> p n t", n=8), axis=AX.X, op=ALU.add),
                 r=[RKe], w=[Rkm])
            P.op("dve", lambda e: e.tensor_reduce(out=km[64:128, 0:8], in_=Ko[64:128, :].rearrange("p (n t) -> p n t", n=8), axis=AX.X, op=ALU.add),
                 r=[RKo], w=[Rkm])
            ts("dve", km[:, 0:8], km[:, 0:8], 1.0 / 256, None, ALU.mult, None, [Rkm], [Rkm])
            cp("dve", kmb[:, 0:8], km[:, 0:8], [Rkm], [Rkmb])
            tt("dve", km[:, 8:16], km[:, 0:8], kmb[:, 0:8], ALU.subtract, [Rkm, Rkmb], [Rkm])
            cp("dve", kmb[:, 8:16], km[:, 8:16], [Rkm], [Rkmb])
            if sub <= 2:
                continue
            for i in range(16):
                mm(psb[2][:, i * 16:(i + 1) * 16], Qe[0:64, i * 128:(i + 1) * 128], kmb[0:64, :], True, True, [RQe, Rkmb], [Rps[2]], skip=True)
                mm(psb[1][:, i * 16:(i + 1) * 16], Qo[64:128, i * 128:(i + 1) * 128], kmb[64:128, :], True, True, [RQo, Rkmb], [Rps[1]], skip=True)
            negA = cst[:, C_NEGA:C_NEGA + 128].rearrange("p (i n) -> p i n", i=16)
            ownA = cst[:, C_OWNA:C_OWNA + 128].rearrange("p (i n) -> p i n", i=16)
            for hh_ in range(2):
                gbk = 2 if hh_ == 0 else 1
                G = psb[gbk][:, 0:256].rearrange("p (i c) -> p i c", i=16)
                g3 = gmA[:, hh_]
                tt("dve", g3, G[:, :, 8:16], negA, ALU.add, [Rps[gbk], Rc], [Rgm])
                tt("dve", g3, G[:, :, 0:8], g3, ALU.add, [Rps[gbk], Rgm], [Rgm])
                for i in range(16):
                    P.op("dve", lambda e, o=topA[:, hh_, i, :], i_=g3[:, i, :]: e.max(out=o, in_=i_), r=[Rgm], w=[Rtop])
                tt("dve", g3, g3, topA[:, hh_, :, 2:3].to_broadcast([128, 16, 8]), ALU.is_ge, [Rgm, Rtop], [Rgm])
                tt("dve", g3, g3, ownA, ALU.max, [Rgm, Rc], [Rgm])
                bcol = 64 if hh_ == 0 else 0
                ts("dve", biasA[:, :, bcol:bcol + 8], g3, BIG, -BIG, ALU.mult, ALU.add, [Rgm], [Rbias])
            for i in range(16):
                c4 = i // 4
                sl = slice((i % 4) * 128, (i % 4) * 128 + 128)
                mm(psb[3][:, sl], biasA[:, i, :], ident_b, True, True, [Rbias, Rcb], [Rps[3]], skip=True)
                if i % 4 == 3:
                    act(Qe[64:72, c4 * 512:(c4 + 1) * 512], psb[3][64:72, :], AF.Identity, [Rps[3]], [RQe])
                    act(Qo[0:8, c4 * 512:(c4 + 1) * 512], psb[3][0:8, :], AF.Identity, [Rps[3]], [RQo])
            if sub <= 3:
                continue
            tiles = []
            for par in range(2):
                for qc in range(4):
                    nk = 4 * qc + 4
                    for kt in range(nk):
                        tiles.append((par, qc, kt, kt == nk - 1))

            def S_step(idx, t):
                par, qc, kt, lastk = t
                Qa, Ka, RQa, RKa = (Qe, Ke, RQe, RKe) if par == 0 else (Qo, Ko, RQo, RKo)
                rows = slice(0, 72) if par == 0 else slice(0, 128)
                q0 = qc * 512
                k0 = kt * 128
                lo = max(q0, k0)
                ncols = q0 + 512 - lo
                sbank = (4, 5, 0, 1)[idx % 4]
                E, RE = Eb[idx % 4], REb[idx % 4]
                sps = psb[sbank][:, 0:ncols]
                mm(sps, Ka[rows, k0:k0 + 128], Qa[rows, lo:q0 + 512], True, True, [RKa, RQa], [Rps[sbank]])
                act(E[:, 0:ncols], sps, AF.Exp, [Rps[sbank]], [RE], scale=0.125)
                if k0 >= q0:
                    tt("dve", E[:, 0:128], E[:, 0:128], tri_b, ALU.mult, [RE, Rcb], [RE])

            def AV_step(idx, t, cpi=cpi):
                par, qc, kt, lastk = t
                h = 2 * cpi + par
                q0 = qc * 512
                k0 = kt * 128
                lo = max(q0, k0)
                E, RE = Eb[idx % 4], REb[idx % 4]
                obank = 6 + (qc % 2)
                for qt in range(4):
                    qs = q0 + qt * 128
                    if qs < k0:
                        continue
                    off = qs - lo
                    ov = psb[obank][:, qt * 66:qt * 66 + 65]
                    mm(ov, E[:, off:off + 128], Vm[:, kt, h, 0:65], (kt == 0 and qt == 0), kt == (qs // 128), [RE, RVm], [Rps[obank]], skip=True)
                if not lastk:
                    return None
                ov = psb[obank][:, 0:264].rearrange("p (a b) -> p a b", a=4)
                rd_ = rden2[qc % 2]
                Rrd_ = Rrden2[qc % 2]
                P.op("dve", lambda e, o=rd_, i_=ov[:, :, 64]: e.reciprocal(out=o, in_=i_), r=[Rps[obank]], w=[Rrd_])
                for qt in range(4):
                    ts("dve", yct[:, qc * 4 + qt, par * 64:(par + 1) * 64], ov[:, qt, 0:64], rd_[:, qt:qt + 1], None, ALU.mult, None,
                       [Rps[obank], Rrd_], [Ryct])
                if par == 0:
                    return None

                def fin(qc=qc, q0=q0, cpi=cpi):
                    pv = psb[3][:, :].bitcast(BF16)
                    half = (qc % 2) * 512
                    for qt in range(4):
                        tr(pv[:, half + qt * 128:half + (qt + 1) * 128], yct[:, qc * 4 + qt, :], ident_b, [Ryct, Rcb], [Rps[3]])
                    act(ycT[:, cpi, q0:q0 + 512], pv[:, half:half + 512], AF.Identity, [Rps[3]], [RycT])
                return [(3, fin)]

            run_pipeline(tiles, S_step, AV_step, 2)
        A.release(m0)

    def build_gateB(l, q, gB, RgB):
        dg = A.alloc([128], F32)
        Rdg = Reg("dg")
        for k in range(8):
            ts("dve", dg, ident_f, modc[:, l, q * 8 + k:q * 8 + k + 1], None, ALU.mult, None, [Rc, Rmod], [Rdg])
            mm(psb[k % 2][:, 0:128], ones_f, dg, True, True, [Rc, Rdg], [Rps[k % 2]])
            cp("dve", gB[:, k * 128:(k + 1) * 128], psb[k % 2][:, 0:128], [Rps[k % 2]], [RgB])

    def phase_merge(l, src):
        P.barrier()
        m0 = A.mark()
        gB = A.alloc([D], F32)
        RgB = Reg("gB")
        build_gateB(l, 2, gB, RgB)
        wo = A.alloc([8, D], BF16)
        Rwo = Reg("wo")
        dma("pool", wo[:, :, 0:512], kmajor(w_out[l], 0, 512), [], [Rwo])
        dma("pool", wo[:, :, 512:1024], kmajor(w_out[l], 512, 512), [], [Rwo])
        wm = [A.alloc([16, 256], BF16) for _ in range(2)]
        Rwm = [Reg("wm%d" % i) for i in range(2)]
        wg = [A.alloc([8, 3, 256], BF16) for _ in range(2)]
        Rwg = [Reg("wg%d" % i) for i in range(2)]
        mT = A.alloc([8, 1024], BF16)
        RmT = Reg("mT")
        gs = [A.alloc([512], BF16) for _ in range(6)]
        Rgs = [Reg("gs%d" % i) for i in range(6)]
        mm_ = [A.alloc([512], F32) for _ in range(4)]
        Rmm = [Reg("mm%d" % i) for i in range(4)]
        rot = {"b": 0, "g": 0, "m": 0}
        xt = [A.alloc([D], F32) for _ in range(2)]
        Rxt = [Reg("xtm%d" % i) for i in range(2)]
        tmp = A.alloc([512], F32)
        Rtmp = Reg("tmpm")
        it = 0
        xit = 0
        for hf in range(2):
            t0 = hf * 1024
            for oc in range(8):
                if oc % 2 == 0:
                    wfull, Rw_ = wm[it % 2], Rwm[it % 2]
                    gfull, Rg_ = wg[it % 2], Rwg[it % 2]
                    it += 1
                    dma("pool", wfull[:, 0:8, :], kmajor(w_br_a[l], oc * 128, 256), [], [Rw_])
                    dma("pool", wfull[:, 8:12, :], kmajor(w_br_b[l], oc * 128, 256), [], [Rw_])
                    dma("pool", wfull[:, 12:16, :], kmajor(w_br_c[l], oc * 128, 256), [], [Rw_])
                    for br in range(3):
                        dma("pool", gfull[:, :, br, :], kmajor(w_in[l], 5120 + br * 1024 + oc * 128, 256), [], [Rg_])
                osl = slice((oc % 2) * 128, (oc % 2) * 128 + 128)
                w_ = wfull[:, :, osl]
                g_ = gfull[:, :, :, osl]
                for cc in range(2):
                    tk = slice(t0 + cc * 512, t0 + (cc + 1) * 512)
                    ci = t0 // 512 + cc
                    gb = []
                    for br in range(3):
                        bk = rot["b"] % 8
                        rot["b"] += 1
                        gb.append(bk)
                        for k in range(8):
                            mm(psb[bk][:, :], g_[:, k, br, :], hT[:, k, tk], k == 0, k == 7, [Rg_, RhT[ci]], [Rps[bk]])
                        gsb, Rgsb = gs[rot["g"] % 6], Rgs[rot["g"] % 6]
                        rot["g"] += 1
                        gb[-1] = (bk, gsb, Rgsb)
                        act(gsb, psb[bk][:, :], AF.Sigmoid, [Rps[bk], Rv], [Rgsb],
                            bias=vecs[:, l, V_GATEB + br * 8 + oc:V_GATEB + br * 8 + oc + 1])
                    ab = []
                    for (nk_, k0_, ysrc, Rys) in ((8, 0, yaT, RyaT[hf]), (4, 8, ybT, RybT), (4, 12, ycT, RycT)):
                        bk = rot["b"] % 8
                        rot["b"] += 1
                        ab.append(bk)
                        for k in range(nk_):
                            mm(psb[bk][:, :], w_[:, k0_ + k, :], ysrc[:, k, tk], k == 0, k == nk_ - 1, [Rw_, Rys], [Rps[bk]])
                    m0_, Rm0_ = mm_[rot["m"] % 4], Rmm[rot["m"] % 4]
                    m1_, Rm1_ = mm_[(rot["m"] + 1) % 4], Rmm[(rot["m"] + 1) % 4]
                    rot["m"] += 2
                    tt("dve", m0_, psb[ab[0]][:, :], gb[0][1], ALU.mult, [Rps[ab[0]], gb[0][2]], [Rm0_])
                    tt("dve", m1_, psb[ab[1]][:, :], gb[1][1], ALU.mult, [Rps[ab[1]], gb[1][2]], [Rm1_])
                    tt("dve", m0_, m0_, m1_, ALU.add, [Rm0_, Rm1_], [Rm0_])
                    tt("dve", m1_, psb[ab[2]][:, :], gb[2][1], ALU.mult, [Rps[ab[2]], gb[2][2]], [Rm1_])
                    tt("dve", mT[:, oc, cc * 512:(cc + 1) * 512], m0_, m1_, ALU.add, [Rm0_, Rm1_], [RmT])
            for il in range(8):
                i = hf * 8 + il
                xb_, Rxb_ = xt[xit % 2], Rxt[xit % 2]
                xit += 1
                dma("sp", xb_, src[i * 128:(i + 1) * 128, :], [Ry[i]], [Rxb_])
                for nh in range(2):
                    bank = rot["b"] % 8
                    rot["b"] += 1
                    for k in range(8):
                        mm(psb[bank][:, :], mT[:, k, il * 128:(il + 1) * 128], wo[:, k, nh * 512:(nh + 1) * 512], k == 0, k == 7,
                           [RmT, Rwo], [Rps[bank]])
                    tt("dve", tmp, psb[bank][:, :], gB[:, nh * 512:(nh + 1) * 512], ALU.mult, [Rps[bank], RgB], [Rtmp])
                    tt("dve", xb_[:, nh * 512:(nh + 1) * 512], xb_[:, nh * 512:(nh + 1) * 512], tmp, ALU.add, [Rxb_, Rtmp], [Rxb_])
                dma("sp", y_d[i * 128:(i + 1) * 128, :], xb_, [Rxb_], [Ry[i]])
        A.release(m0)

    def phase_ffn(l, experts):
        P.barrier()
        m0 = A.mark()
        gB = A.alloc([D], F32)
        RgB = Reg("gB2")
        build_gateB(l, 5, gB, RgB)
        moe = experts[0][3] is not None
        comb = None
        if moe:
            comb = A.alloc([16, 8], F32)
            Rcomb = Reg("comb")
            rwb = A.alloc([64], BF16)
            Rrwb = Reg("rwb")
            dma("pool", rwb, rw_d[:, :], [], [Rrwb])
            lg = A.alloc([8], F32)
            Rlg = Reg("lg")
            tp = A.alloc([8], F32)
            Rtp = Reg("tp")
            wts = A.alloc([4], F32)
            Rwts = Reg("wts")
            msk = A.alloc([8], F32)
            Rmsk = Reg("msk")
            rwv = rwb.rearrange("p (k e) -> p k e", k=8)
            for i in range(16):
                for k in range(8):
                    mm(psb[0][:, 0:8], hT[:, k, i * 128:(i + 1) * 128], rwv[:, k, :], k == 0, k == 7, [RhT[i // 4], Rrwb], [Rps[0]])
                tt("dve", lg, psb[0][:, 0:8], vecs[:, l, V_RB:V_RB + 8], ALU.add, [Rps[0], Rv], [Rlg])
                P.op("dve", lambda e: e.max(out=tp, in_=lg), r=[Rlg], w=[Rtp])
                tt("dve", wts[:, 0:1], tp[:, 0:1], tp[:, 1:2], ALU.subtract, [Rtp], [Rwts])
                act(wts[:, 1:2], wts[:, 0:1], AF.Sigmoid, [Rwts], [Rwts])
                ts("dve", wts[:, 2:3], wts[:, 1:2], -1.0, 1.0, ALU.mult, ALU.add, [Rwts], [Rwts])
                ts("dve", msk, lg, tp[:, 0:1], wts[:, 1:2], ALU.is_equal, ALU.mult, [Rlg, Rtp, Rwts], [Rmsk])
                ts("dve", comb[:, i, :], lg, tp[:, 1:2], wts[:, 2:3], ALU.is_equal, ALU.mult, [Rlg, Rtp, Rwts], [Rcomb])
                tt("dve", comb[:, i, :], comb[:, i, :], msk, ALU.add, [Rcomb, Rmsk], [Rcomb])
        HT = 1024
        uT = A.alloc([NFC, HT], BF16)
        RuT = [Reg("uT%d" % c) for c in range(2)]
        w13 = [A.alloc([2, 8, 256], BF16) for _ in range(2)]
        Rw13 = [Reg("w13_%d" % i) for i in range(2)]
        w2b = [A.alloc([NFC, 512], BF16) for _ in range(3)]
        Rw2b = [Reg("w2b%d" % i) for i in range(3)]
        sl_ = [A.alloc([512], F32) for _ in range(2)]
        Rsl = [Reg("silu%d" % i) for i in range(2)]
        tmp = [A.alloc([512], F32) for _ in range(2)]
        Rtmp = [Reg("tmpf%d" % i) for i in range(2)]
        ybuf = [A.alloc([512], F32) for _ in range(3)]
        Rybuf = [Reg("ybuf%d" % i) for i in range(3)]
        wit = 0
        w2it = 0
        pit = 0
        sit = 0
        tit = 0
        groups = [(c0, 256) for c0 in range(0, DFF, 256)]
        for hf in range(2):
            t0 = hf * HT
            for (w1, w3, w2, e) in experts:
                w2slots = []
                for gi, (c0, nc_) in enumerate(groups):
                    wb, Rwb = w13[wit % 2], Rw13[wit % 2]
                    wit += 1
                    dma("pool", wb[:, 0, :, 0:nc_], kmajor(w1, c0, nc_), [], [Rwb])
                    dma("pool", wb[:, 1, :, 0:nc_], kmajor(w3, c0, nc_), [], [Rwb])
                    if gi in (2, 5):
                        nh = 0 if gi == 2 else 1
                        w2t, Rw2t = w2b[w2it % 3], Rw2b[w2it % 3]
                        w2it += 1
                        src2 = w2[:, nh * 512:(nh + 1) * 512].rearrange("(f p) n -> p f n", p=128)
                        dma("pool", w2t[:, 0:11, :], src2[:, 0:11, :], [], [Rw2t])
                        dma("pool", w2t[:, 11:22, :], src2[:, 11:22, :], [], [Rw2t])
                        w2slots.append((w2t, Rw2t))
                    for fcl in range(nc_ // 128):
                        f = c0 // 128 + fcl
                        for cc in range(2):
                            b1 = (pit % 2) * 2
                            pit += 1
                            for g in range(2):
                                for k in range(8):
                                    mm(psb[b1 + g][:, :], wb[:, g, k, fcl * 128:(fcl + 1) * 128], hT[:, k, t0 + cc * 512:t0 + (cc + 1) * 512],
                                       k == 0, k == 7, [Rwb, RhT[t0 // 512 + cc]], [Rps[b1 + g]])
                            s_, Rs_ = sl_[sit % 2], Rsl[sit % 2]
                            sit += 1
                            act(s_, psb[b1][:, :], AF.Silu, [Rps[b1]], [Rs_])
                            tt("dve", uT[:, f, cc * 512:(cc + 1) * 512], psb[b1 + 1][:, :], s_, ALU.mult, [Rps[b1 + 1], Rs_], [RuT[cc]])
                for nh in range(2):
                    w2t, Rw2t = w2slots[nh]
                    for il in range(8):
                        i = hf * 8 + il
                        bank = 4 + (pit % 4)
                        pit += 1
                        for f in range(NFC):
                            mm(psb[bank][:, :], uT[:, f, il * 128:(il + 1) * 128], w2t[:, f, :], f == 0, f == NFC - 1,
                               [RuT[il // 4], Rw2t], [Rps[bank]])
                        t_, Rt_ = tmp[tit % 2], Rtmp[tit % 2]
                        tit += 1
                        if moe:
                            stt(t_, psb[bank][:, :], comb[:, i, e:e + 1], gB[:, nh * 512:(nh + 1) * 512], ALU.mult, ALU.mult,
                                [Rps[bank], Rcomb, RgB], [Rt_])
                        else:
                            tt("dve", t_, psb[bank][:, :], gB[:, nh * 512:(nh + 1) * 512], ALU.mult, [Rps[bank], RgB], [Rt_])
                        yb_, Ryb_ = ybuf[tit % 3], Rybuf[tit % 3]
                        ysl = y_d[i * 128:(i + 1) * 128, nh * 512:(nh + 1) * 512]
                        dma("sp", yb_, ysl, [Ry[i]], [Ryb_])
                        tt("dve", yb_, yb_, t_, ALU.add, [Ryb_, Rt_], [Ryb_])
                        dma("sp", ysl, yb_, [Ryb_], [Ry[i]])
        A.release(m0)

    def dump(name, ap, shape, dt, regs):
        d = dbg_out(name, shape, dt)
        P.barrier()
        dma("sp", d, ap, regs, [])

    for l in range(2):
        src = x_d if l == 0 else y_d
        phase_norm(l, 0, src)
        if stage_end("norm1_%d" % l):
            dump("hT", hT, [128, 8, S], BF16, RhT)
            break
        P.barrier()
        A.release(cs_mark)
        A.alloc([S], F32)
        A.alloc([S], F32)
        compute_rope()
        phase_diff(l)
        if stage_end("diff_%d" % l):
            dump("ybT", ybT, [128, 4, S], BF16, [RybT])
            break
        phase_moba(l)
        if stage_end("moba_%d" % l):
            dump("ycT", ycT, [128, 4, S], BF16, [RycT])
            break
        A.release(cs_mark)
        phase_rnn(l)
        if stage_end("rnn_%d" % l):
            dump("yaT", yaT, [128, 8, S], BF16, RyaT)
            break
        phase_merge(l, src)
        if stage_end("merge_%d" % l):
            break
        A.release(base_mark)
        phase_norm(l, 1, y_d)
        if stage_end("norm2_%d" % l):
            dump("hT", hT, [128, 8, S], BF16, RhT)
            break
        if l == 0:
            phase_ffn(l, [(ffn_w1[0], ffn_w3[0], ffn_w2[0], None)])
        else:
            phase_ffn(l, [(moe_w1[0, e], moe_w3[0, e], moe_w2[0, e], e) for e in range(8)])
        if stage_end("ffn_%d" % l):
            break
        A.release(base_mark)
    P.finish()
    return nc, es, P, A, dbg_outs


_CACHE = {}


def prep_inputs(inp, b):
    pos = np.ascontiguousarray(np.broadcast_to(np.asarray(inp["positions"])[b][None, :], (128, S))).astype(np.int32)
    oh = np.zeros((8, S), np.float32)
    for n in range(8):
        oh[n, n * 256:(n + 1) * 256] = 1.0
    rw = np.ascontiguousarray(np.asarray(inp["router_w"])[0].reshape(8, 128, 8).transpose(1, 0, 2).reshape(128, 64))
    return {
        "x": np.ascontiguousarray(np.asarray(inp["x"])[b]),
        "cT": colform(np.asarray(inp["c"])[b], 8),
        "pos": pos,
        "onehot": oh,
        "rw": rw,
    }


SHARED = ["ada_w", "w_in", "lru_wa", "lru_wx", "w_br_a", "w_br_b", "w_br_c", "w_out", "ffn_w1", "ffn_w3", "ffn_w2",
          "moe_w1", "moe_w3", "moe_w2"]


def kernel(**inputs):
    inp = {k: np.asarray(v) for k, v in inputs.items()}
    if "prog" not in _CACHE:
        _CACHE["prog"] = build_program()
    nc = _CACHE["prog"][0]
    consts = make_consts()
    vecs = make_vecs(inp)
    shared = {k: np.ascontiguousarray(inp[k], dtype=np.float32) for k in SHARED}
    in_maps = []
    for b in range(8):
        m = prep_inputs(inp, b)
        m["consts"] = consts
        m["vecs"] = vecs
        m.update(shared)
        in_maps.append(m)
    res = run_bass_kernel_spmd(nc, in_maps, core_ids=list(range(8)))
    out = np.stack([np.asarray(r["y"]) for r in res.results], axis=0).astype(np.float32)
    return out
```
